# Optimizing a Trainium2 kernel written in Bass

```python
import math
import jax, jax.numpy as jnp
from jax import lax
import numpy as np

D_MODEL = 1024
BATCH = 16
SEQ = 2048
DEPTH = 1

HG_HEADS = 4
HG_KDIM = 128
HG_VDIM = 128
HG_KWIDTH = HG_HEADS * HG_KDIM
HG_WIDTH = HG_HEADS * HG_VDIM
DA_HEADS = 4
DA_HEAD_DIM = 64
DA_WIDTH = DA_HEADS * 2 * DA_HEAD_DIM
MIX_WIDTH = HG_WIDTH + DA_WIDTH
IN_SIZES = (HG_KWIDTH, HG_KWIDTH, HG_WIDTH, HG_WIDTH, DA_WIDTH, DA_WIDTH, DA_WIDTH)
IN_WIDTH = sum(IN_SIZES)
D_FF = -(-8 * D_MODEL // (3 * 256)) * 256
CHUNK = 64
Q_BLOCK = 128
EPS = 1e-6
N_MOD = 6

kernel_name = "hymba_hgrn2_diffattn_alibi_adaln_block"


def rmsnorm(x, w):
    x32 = x.astype(jnp.float32)
    y = x32 * lax.rsqrt(jnp.mean(x32 * x32, axis=-1, keepdims=True) + EPS)
    return (y * w.astype(jnp.float32)).astype(x.dtype)


def alibi_slopes(n):
    return jnp.asarray(np.array([2.0 ** (-8.0 * (h + 1) / n) for h in range(n)], dtype=np.float32))


def hgrn2(q, f_logit, i, g, lb, gnorm_w):
    B, T, _ = q.shape
    n = T // CHUNK
    f = lb + (1.0 - lb) * jax.nn.sigmoid(f_logit.astype(jnp.float32))
    logf = jnp.log(f)
    k = 1.0 - f

    def heads(t, d):
        return t.reshape(B, n, CHUNK, HG_HEADS, d).transpose(1, 0, 3, 2, 4)

    qc = heads(q.astype(jnp.float32), HG_KDIM)
    kc = heads(k, HG_KDIM)
    vc = heads(i.astype(jnp.float32), HG_VDIM)
    bc = jnp.cumsum(heads(logf, HG_KDIM), axis=3)
    causal = jnp.tril(jnp.ones((CHUNK, CHUNK), dtype=bool))[:, :, None]

    def step(S, inp):
        qb, kb, vb, bb = inp
        o_inter = jnp.einsum('bhtk,bhkv->bhtv', qb * jnp.exp(bb), S)
        rel = bb[:, :, :, None, :] - bb[:, :, None, :, :]
        decay = jnp.exp(jnp.where(causal, rel, -jnp.inf))
        A = jnp.einsum('bhtk,bhsk,bhtsk->bhts', qb, kb, decay)
        o = o_inter + jnp.einsum('bhts,bhsv->bhtv', A, vb)
        blast = bb[:, :, -1:, :]
        S_new = jnp.exp(blast[:, :, 0, :])[..., None] * S + jnp.einsum('bhsk,bhsv->bhkv', kb * jnp.exp(blast - bb), vb)
        return S_new, o

    S0 = jnp.zeros((B, HG_HEADS, HG_KDIM, HG_VDIM), jnp.float32)
    _, o = lax.scan(step, S0, (qc, kc, vc, bc))
    o = o.transpose(1, 0, 3, 2, 4).reshape(B, T, HG_HEADS, HG_VDIM)
    o = rmsnorm(o, gnorm_w) * jax.nn.silu(g.reshape(B, T, HG_HEADS, HG_VDIM).astype(jnp.float32))
    return o.reshape(B, T, HG_WIDTH).astype(q.dtype)


def diff_attention(q, k, v, lam, lambda_init, subln_w):
    B, T, _ = q.shape
    d = DA_HEAD_DIM
    q = q.reshape(B, T, DA_HEADS, 2, d).transpose(0, 2, 3, 1, 4)
    k = k.reshape(B, T, DA_HEADS, 2, d).transpose(0, 2, 3, 1, 4)
    v = v.reshape(B, T, DA_HEADS, 2 * d).transpose(0, 2, 1, 3)
    scale = d ** -0.5
    slopes = alibi_slopes(DA_HEADS)
    pos = jnp.arange(T)
    outs = []
    for blk in range(T // Q_BLOCK):
        lo, hi = blk * Q_BLOCK, (blk + 1) * Q_BLOCK
        qb, kb, vb = q[:, :, :, lo:hi], k[:, :, :, :hi], v[:, :, :hi]
        s = jnp.einsum('bhmqd,bhmkd->bhmqk', qb, kb).astype(jnp.float32) * scale
        dist = (pos[lo:hi, None] - pos[None, :hi]).astype(jnp.float32)
        s = s - (slopes[:, None, None] * dist)[None, :, None]
        s = jnp.where(dist >= 0, s, -jnp.inf)
        p = jax.nn.softmax(s, axis=-1)
        w = p[:, :, 0] - lam * p[:, :, 1]
        outs.append(jnp.einsum('bhqk,bhkv->bhqv', w.astype(v.dtype), vb))
    o = jnp.concatenate(outs, axis=2)
    o = rmsnorm(o, subln_w) * (1.0 - lambda_init)
    return o.transpose(0, 2, 1, 3).reshape(B, T, DA_WIDTH)


def setup_inputs(seed: int = 0) -> dict:
    key = jax.random.key(seed)
    ks = jax.random.split(key, 20)
    f32 = jnp.float32
    nrm = lambda k, shape, s: jax.random.normal(k, shape, f32) * s
    return {
        "x": nrm(ks[0], (BATCH, SEQ, D_MODEL), 1.0),
        "c": nrm(ks[1], (BATCH, D_MODEL), 1.0),
        "w_ada": nrm(ks[2], (DEPTH, D_MODEL, N_MOD * D_MODEL), 0.5 * D_MODEL ** -0.5),
        "b_ada": nrm(ks[3], (DEPTH, N_MOD * D_MODEL), 0.01),
        "norm1_w": 1.0 + nrm(ks[4], (DEPTH, D_MODEL), 0.02),
        "w_in": nrm(ks[5], (DEPTH, D_MODEL, IN_WIDTH), D_MODEL ** -0.5),
        "hgrn_lb_logits": nrm(ks[6], (DEPTH + 1, HG_KWIDTH), 0.5),
        "hgrn_gnorm_w": 1.0 + nrm(ks[7], (DEPTH, HG_VDIM), 0.02),
        "diff_lambda_q1": nrm(ks[8], (DEPTH, DA_HEAD_DIM), 0.1),
        "diff_lambda_k1": nrm(ks[9], (DEPTH, DA_HEAD_DIM), 0.1),
        "diff_lambda_q2": nrm(ks[10], (DEPTH, DA_HEAD_DIM), 0.1),
        "diff_lambda_k2": nrm(ks[11], (DEPTH, DA_HEAD_DIM), 0.1),
        "diff_subln_w": 1.0 + nrm(ks[12], (DEPTH, 2 * DA_HEAD_DIM), 0.02),
        "w_out": nrm(ks[13], (DEPTH, MIX_WIDTH, D_MODEL), MIX_WIDTH ** -0.5),
        "norm2_w": 1.0 + nrm(ks[14], (DEPTH, D_MODEL), 0.02),
        "w_ffn_gate": nrm(ks[15], (DEPTH, D_MODEL, D_FF), D_MODEL ** -0.5),
        "w_ffn_up": nrm(ks[16], (DEPTH, D_MODEL, D_FF), D_MODEL ** -0.5),
        "w_ffn_down": nrm(ks[17], (DEPTH, D_FF, D_MODEL), D_FF ** -0.5),
        "final_norm_w": 1.0 + nrm(ks[18], (D_MODEL,), 0.02),
    }


def reference(x, c, w_ada, b_ada, norm1_w, w_in, hgrn_lb_logits, hgrn_gnorm_w,
              diff_lambda_q1, diff_lambda_k1, diff_lambda_q2, diff_lambda_k2, diff_subln_w,
              w_out, norm2_w, w_ffn_gate, w_ffn_up, w_ffn_down, final_norm_w):
    offs = np.cumsum(IN_SIZES)[:-1].tolist()
    lower_bounds = jnp.cumsum(jax.nn.softmax(hgrn_lb_logits.astype(jnp.float32), axis=0), axis=0)
    h = x
    for l in range(DEPTH):
        mod = jax.nn.silu(c) @ w_ada[l] + b_ada[l]
        sh1, sc1, g1, sh2, sc2, g2 = jnp.split(mod[:, None, :], N_MOD, axis=-1)
        u = rmsnorm(h, norm1_w[l]) * (1.0 + sc1) + sh1
        proj = u @ w_in[l]
        hq, hf, hi, hg, aq, ak, av = jnp.split(proj, offs, axis=-1)
        o_hg = hgrn2(hq, hf, hi, hg, lower_bounds[l], hgrn_gnorm_w[l])
        lambda_init = 0.8 - 0.6 * math.exp(-0.3 * l)
        lam = (jnp.exp(jnp.sum(diff_lambda_q1[l].astype(jnp.float32) * diff_lambda_k1[l].astype(jnp.float32)))
               - jnp.exp(jnp.sum(diff_lambda_q2[l].astype(jnp.float32) * diff_lambda_k2[l].astype(jnp.float32)))
               + lambda_init)
        o_da = diff_attention(aq, ak, av, lam, lambda_init, diff_subln_w[l])
        mix = jnp.concatenate([o_hg, o_da.astype(o_hg.dtype)], axis=-1) @ w_out[l]
        h = h + g1 * mix
        u = rmsnorm(h, norm2_w[l]) * (1.0 + sc2) + sh2
        ff = (jax.nn.silu(u @ w_ffn_gate[l]) * (u @ w_ffn_up[l])) @ w_ffn_down[l]
        h = h + g2 * ff
    return rmsnorm(h, final_norm_w)
```

```python
import numpy as np
import concourse.bass as bass
import concourse.mybir as mybir
from concourse.bass_utils import run_bass_kernel_spmd

F32 = mybir.dt.float32
BF16 = mybir.dt.bfloat16
AF = mybir.ActivationFunctionType
ALU = mybir.AluOpType
AX = mybir.AxisListType

D = 1024
DFF = 2816
NKF = DFF // 128
INW = 3584
ENGS = ("pe", "act", "dve", "pool", "sp")


class Op:
    __slots__ = ("eng", "fn", "reads", "writes", "dma", "waits", "mark", "seq", "dsem", "dval", "idx", "fence")

    def __init__(self, eng, fn, reads, writes, dma):
        self.eng = eng
        self.fn = fn
        self.reads = tuple(reads)
        self.writes = tuple(writes)
        self.dma = dma
        self.waits = []
        self.mark = False
        self.seq = 0
        self.dsem = None
        self.dval = 0
        self.fence = False


class Prog:
    def __init__(self, n_dma_sems=24):
        self.ops = []
        self.n_dma_sems = n_dma_sems

    def op(self, eng, fn, reads=(), writes=(), dma=False):
        o = Op(eng, fn, reads, writes, dma)
        o.idx = len(self.ops)
        self.ops.append(o)
        return o

    def fence(self):
        o = Op("sp", None, (), (), False)
        o.idx = len(self.ops)
        o.fence = True
        self.ops.append(o)

    def analyze(self):
        last_writer = {}
        readers = {}
        need = []
        last_eng = {e: None for e in ENGS}
        pend_dma = []
        fence_deps = []
        for o in self.ops:
            if o.fence:
                fence_deps = [j for j in last_eng.values() if j is not None] + list(pend_dma)
                pend_dma = []
                last_writer = {}
                readers = {}
                need.append([])
                continue
            deps = {}
            for j in fence_deps:
                deps[j] = "fence"
            for r in o.reads:
                j = last_writer.get(r)
                if j is not None:
                    deps[j] = "raw"
            for w in o.writes:
                j = last_writer.get(w)
                if j is not None and j not in deps:
                    deps[j] = "waw"
                for j in readers.get(w, ()):
                    if j not in deps and j != o.idx:
                        deps[j] = "war"
            for r in o.reads:
                readers.setdefault(r, []).append(o.idx)
            for w in o.writes:
                last_writer[w] = o.idx
                readers[w] = []
            nd = []
            for j, kind in deps.items():
                p = self.ops[j]
                if p.dma:
                    nd.append(j)
                elif p.eng == o.eng:
                    if o.dma:
                        p.mark = True
                        nd.append(j)
                    elif p.eng == "pe":
                        continue
                    elif kind != "fence":
                        p.mark = True
                        nd.append(j)
                else:
                    p.mark = True
                    nd.append(j)
            need.append(nd)
            if o.dma:
                pend_dma.append(o.idx)
            else:
                last_eng[o.eng] = o.idx
        cnt = {e: 0 for e in ENGS}
        dcnt = [0] * self.n_dma_sems
        k = 0
        kp = 0
        for o in self.ops:
            if o.fence:
                continue
            if o.dma:
                if o.eng == "pool":
                    o.dsem = kp % 8
                    kp += 1
                else:
                    o.dsem = 8 + k % (self.n_dma_sems - 8)
                    k += 1
                dcnt[o.dsem] += 1
                o.dval = 16 * dcnt[o.dsem]
            elif o.mark:
                cnt[o.eng] += 1
                o.seq = cnt[o.eng]
        waited = {e: {} for e in ENGS}
        for o in self.ops:
            if o.fence:
                continue
            w = {}
            for j in need[o.idx]:
                p = self.ops[j]
                if p.dma:
                    key, val = ("d", p.dsem), p.dval
                else:
                    key, val = ("e", p.eng), p.seq
                if val > w.get(key, 0):
                    w[key] = val
            if o.dma and o.dval > 16:
                key = ("d", o.dsem)
                w[key] = max(w.get(key, 0), o.dval - 16)
            ws = waited[o.eng]
            for key, val in w.items():
                if ws.get(key, 0) < val:
                    ws[key] = val
                    o.waits.append((key, val))
        self.final_dma = [(i, 16 * c) for i, c in enumerate(dcnt) if c > 0]
        self.final_cnt = cnt

    def emit(self, block, esems, dsems):
        def run_engine(ename, eng):
            for o in self.ops:
                if o.fence or o.eng != ename:
                    continue
                for key, val in o.waits:
                    sem = dsems[key[1]] if key[0] == "d" else esems[key[1]]
                    eng.wait_ge(sem, val)
                ins = o.fn(eng)
                if o.dma:
                    ins.then_inc(dsems[o.dsem], 16)
                elif o.mark:
                    ins.then_inc(esems[o.eng], 1)
            if ename == "sp":
                for i, v in self.final_dma:
                    eng.wait_ge(dsems[i], v)
                for e in ENGS:
                    if e != "sp" and self.final_cnt[e] > 0:
                        eng.wait_ge(esems[e], self.final_cnt[e])

        block.tensor(lambda e: run_engine("pe", e))
        block.scalar(lambda e: run_engine("act", e))
        block.vector(lambda e: run_engine("dve", e))
        block.gpsimd(lambda e: run_engine("pool", e))
        block.sync(lambda e: run_engine("sp", e))


class Arena:
    def __init__(self, nc, base=16640, limit=229376):
        self.nc = nc
        self.off = base
        self.limit = limit
        self.n = 0

    def alloc(self, name, shape, dt):
        isz = 2 if dt == BF16 else 4
        size = isz
        for s in shape[1:]:
            size *= s
        size = (size + 63) // 64 * 64
        off = self.off
        assert off + size <= self.limit, ("SBUF overflow", name, off, size)
        self.off += size
        self.n += 1
        return self.nc.alloc_sbuf_tensor_at(name, list(shape), dt, offset=off)


def build(T=2048, NSEQ=2, TB=512, TBB=256):
    NSUB = TB // 128
    NBLK = T // TB
    NCH = TB // 64
    NKB = T // 128
    NTOK = NSEQ * T
    nc = bass.Bass("TRN2", target_bir_lowering=False)

    def din(name, shape):
        return nc.dram_tensor(name, list(shape), F32, kind="ExternalInput").ap()

    x = din("x", [NTOK, D])
    c_in = din("c", [NSEQ * 8, 128])
    w_ada = din("w_ada", [D, 6 * D])
    b_ada = din("b_ada", [6 * D])
    n1w = din("norm1_w", [8, 128])
    w_in = din("w_in", [D, INW])
    lbl = din("lb_logits", [8, 128])
    gnw = din("gnorm_w", [128])
    lq1 = din("lq1", [64])
    lk1 = din("lk1", [64])
    lq2 = din("lq2", [64])
    lk2 = din("lk2", [64])
    subw = din("subln_w", [128])
    w_out = din("w_out", [D, D])
    n2w = din("norm2_w", [8, 128])
    w_g = din("w_g", [D, DFF])
    w_u = din("w_u", [D, DFF])
    w_d = din("w_d", [DFF, D])
    fnw = din("fnw", [D])
    out = nc.dram_tensor("out", [NTOK, D], F32, kind="ExternalOutput").ap()
    h1 = nc.dram_tensor("h1_scratch", [NTOK, D], F32, kind="Internal").ap()

    P = Prog()
    A = Arena(nc)

    pbs = [nc.alloc_psum_tensor("pb%d" % i, [128, 512], F32) for i in range(8)]

    def pbank(i):
        return pbs[i][:]

    def MM(out_, lhsT, rhs, start, stop, reads, writes, **kw):
        P.op("pe", lambda e: e.matmul(out_, lhsT=lhsT, rhs=rhs, start=start, stop=stop, **kw), reads, writes)

    def TR(out_, in_, ident, reads, writes):
        P.op("pe", lambda e: e.transpose(out=out_, in_=in_, identity=ident), reads, writes)

    def ACT(out_, in_, func, reads, writes, bias=0.0, scale=1.0, accum_out=None):
        if accum_out is None:
            P.op("act", lambda e: e.activation(out=out_, in_=in_, func=func, bias=bias, scale=scale), reads, writes)
        else:
            P.op("act", lambda e: e.activation(out=out_, in_=in_, func=func, bias=bias, scale=scale, accum_out=accum_out), reads, writes)

    def TS(eng, out_, in0, s1, s2, op0, op1, reads, writes):
        if s2 is None:
            P.op(eng, lambda e: e.tensor_scalar(out=out_, in0=in0, scalar1=s1, scalar2=None, op0=op0), reads, writes)
        else:
            P.op(eng, lambda e: e.tensor_scalar(out=out_, in0=in0, scalar1=s1, scalar2=s2, op0=op0, op1=op1), reads, writes)

    def TT(eng, out_, in0, in1, op, reads, writes):
        P.op(eng, lambda e: e.tensor_tensor(out=out_, in0=in0, in1=in1, op=op), reads, writes)

    def STT(eng, out_, in0, scalar, in1, op0, op1, reads, writes):
        P.op(eng, lambda e: e.scalar_tensor_tensor(out=out_, in0=in0, scalar=scalar, in1=in1, op0=op0, op1=op1), reads, writes)

    def CP(eng, out_, in_, reads, writes):
        if eng == "act":
            P.op("act", lambda e: e.copy(out=out_, in_=in_), reads, writes)
        else:
            P.op(eng, lambda e: e.tensor_copy(out=out_, in_=in_), reads, writes)

    def RECIP(out_, in_, reads, writes):
        P.op("dve", lambda e: e.reciprocal(out=out_, in_=in_), reads, writes)

    def MEMSET(eng, ap, val, writes):
        P.op(eng, lambda e: e.memset(ap, val), (), writes)

    def DMA(out_, in_, reads, writes, eng="sp", **kw):
        P.op(eng, lambda e: e.dma_start(out=out_, in_=in_, **kw), reads, writes, dma=True)

    def rstd_from_ssq(ssq, rstd, n, key):
        TS("dve", rstd, ssq, 1.0 / n, 1e-6, ALU.mult, ALU.add, [key + "ssq"], [key + "rstd"])
        ACT(rstd, rstd, AF.Ln, [key + "rstd"], [key + "rstd"])
        ACT(rstd, rstd, AF.Exp, [key + "rstd"], [key + "rstd"], scale=-0.5)

    ident_f = A.alloc("ident_f", [128, 128], F32)
    ident_b = A.alloc("ident_b", [128, 128], BF16)
    ones_f = A.alloc("ones_f", [128, 128], F32)
    mask_f = A.alloc("mask_f", [128, 128], F32)
    mask_b = A.alloc("mask_b", [128, 128], BF16)
    NST = 8 * NSEQ + 8 + 8 + 48 + 8
    O_C, O_N1, O_N2, O_BA, O_LB = 0, 8 * NSEQ, 8 * NSEQ + 8, 8 * NSEQ + 16, 8 * NSEQ + 64
    stg = A.alloc("stg", [128, 128], F32)
    stgT = A.alloc("stgT", [128, NST], F32)
    modT = A.alloc("modT", [128, 6, 8, NSEQ], F32)
    scl = A.alloc("scl", [128, 2, 8, NSEQ], F32)
    silf = A.alloc("silf", [128, NSEQ * 8], F32)
    siltmp = A.alloc("siltmp", [128, NSEQ * 8], F32)
    scT_b = A.alloc("scT_b", [128, 8, NSEQ], BF16)
    screp = [A.alloc("screp%d" % s, [128, 8, 128], BF16) for s in range(NSEQ)]
    gb = [A.alloc("gb%d" % s, [128, D], F32) for s in range(NSEQ)]
    bada_b = A.alloc("bada_b", [128, D], F32)
    ssq = A.alloc("ssq", [128, 4], F32)
    rstd = A.alloc("rstd", [128, 4], F32)
    phase_base = A.off

    MEMSET("pool", ident_f[:], 0.0, ["ident_f"])
    P.op("pool", lambda e: e.affine_select(out=ident_f[:], in_=ident_f[:], pattern=[[-1, 128]], compare_op=ALU.not_equal,
                                           fill=1.0, base=0, channel_multiplier=1), ["ident_f"], ["ident_f"])
    CP("pool", ident_b[:], ident_f[:], ["ident_f"], ["ident_b"])
    MEMSET("pool", ones_f[:], 1.0, ["ones_f"])
    P.op("pool", lambda e: e.affine_select(out=mask_f[:], in_=ones_f[:], pattern=[[1, 128]], compare_op=ALU.is_ge,
                                           fill=0.0, base=0, channel_multiplier=-1), ["ones_f"], ["mask_f"])
    CP("pool", mask_b[:], mask_f[:], ["mask_f"], ["mask_b"])
    MEMSET("dve", stg[:], 0.0, ["stg"])
    DMA(stg[O_C:O_C + 8 * NSEQ, :], c_in[:, :], [], ["stg"])
    DMA(stg[O_N1:O_N1 + 8, :], n1w[:, :], [], ["stg"])
    DMA(stg[O_N2:O_N2 + 8, :], n2w[:, :], [], ["stg"])
    DMA(stg[O_BA:O_BA + 48, :], b_ada.rearrange("(r p) -> r p", p=128), [], ["stg"])
    DMA(stg[O_LB:O_LB + 8, :], lbl[:, :], [], ["stg"])
    TR(pbank(4)[:, 0:128], stg[:], ident_f[:], ["stg", "ident_f"], ["pb4"])
    CP("dve", stgT[:], pbank(4)[:, 0:NST], ["pb4"], ["stgT"])
    cT = stgT[:, O_C:O_C + 8 * NSEQ]
    ACT(siltmp[:], cT, AF.Exp, ["stgT"], ["siltmp"], scale=-1.0)
    TS("dve", siltmp[:], siltmp[:], 1.0, None, ALU.add, None, ["siltmp"], ["siltmp"])
    RECIP(siltmp[:], siltmp[:], ["siltmp"], ["siltmp"])
    TT("dve", silf[:], cT, siltmp[:], ALU.mult, ["stgT", "siltmp"], ["silf"])
    CP("dve", scT_b[:].rearrange("p k s -> p s k"), silf[:].rearrange("p (s k) -> p s k", k=8), ["silf"], ["scT_b"])
    for s in range(NSEQ):
        for k in range(8):
            TS("dve", screp[s][:, k, :], ones_f[:], silf[:, s * 8 + k:s * 8 + k + 1], None, ALU.mult, None,
               ["ones_f", "silf"], ["screp%d" % s])

    def ada_part(chunks_feat, chunk_row, wada_t):
        for j in list(chunks_feat) + [chunk_row]:
            for half in range(2):
                DMA(wada_t[half][:], w_ada[:, j * D + half * 512: j * D + half * 512 + 512].rearrange("(k p) n -> p k n", p=128),
                    [], ["wada%d" % half], eng="pool")
            if j != chunk_row:
                for cb in range(8):
                    half, lc = cb // 4, (cb % 4) * 128
                    col = (j * 8 + cb) * NSEQ
                    for k in range(8):
                        MM(pbank(4)[:, col:col + NSEQ], wada_t[half][:, k, lc:lc + 128], scT_b[:, k, :], k == 0, k == 7,
                           ["wada%d" % half, "scT_b"], ["pb4"])
                for cb in range(8):
                    col = (j * 8 + cb) * NSEQ
                    TS("dve", modT[:, j, cb, :], pbank(4)[:, col:col + NSEQ], stgT[:, O_BA + j * 8 + cb:O_BA + j * 8 + cb + 1], None,
                       ALU.add, None, ["pb4", "stgT"], ["modT"])
            else:
                DMA(bada_b[:], b_ada[j * D:(j + 1) * D].partition_broadcast(128), [], ["bada_b"])
                for s in range(NSEQ):
                    for half in range(2):
                        for k in range(8):
                            MM(pbank(half)[:], screp[s][:, k, :], wada_t[half][:, k, :], k == 0, k == 7,
                               ["screp%d" % s, "wada%d" % half], ["pb%d" % half])
                        TT("dve", gb[s][:, half * 512:(half + 1) * 512], pbank(half)[:], bada_b[:, half * 512:(half + 1) * 512], ALU.add,
                           ["pb%d" % half, "bada_b"], ["gb%d" % s])

    def make_scl(which, jsc, onw):
        for cb in range(8):
            TS("dve", scl[:, which, cb, :], modT[:, jsc, cb, :], 1.0, stgT[:, onw + cb:onw + cb + 1], ALU.add, ALU.mult,
               ["modT", "stgT"], ["scl"])

    def norm_to_uT(src_tile, s, which, jsh, uT_, col0, xn_, key):
        ACT(xn_[:], src_tile, AF.Square, [key], ["xn", "ssq"], accum_out=ssq[:, 0:1])
        rstd_from_ssq(ssq[:, 0:1], rstd[:, 0:1], D, "")
        TS("dve", xn_[:], src_tile, rstd[:, 0:1], None, ALU.mult, None, [key, "rstd"], ["xn"])
        for g in range(2):
            for k in range(g * 4, g * 4 + 4):
                TR(pbank(4)[:, (k % 4) * 128:(k % 4 + 1) * 128], xn_[:, k * 128:(k + 1) * 128], ident_f[:], ["xn", "ident_f"], ["pb4"])
            for k in range(g * 4, g * 4 + 4):
                ACT(uT_[:, k, col0:col0 + 128], pbank(4)[:, (k % 4) * 128:(k % 4 + 1) * 128], AF.Identity, ["pb4", "scl", "modT"], ["uT"],
                    scale=scl[:, which, k, s:s + 1], bias=modT[:, jsh, k, s:s + 1])

    A.off = phase_base
    w_in_b = A.alloc("w_in_b", [128, 8, INW], BF16)
    w_out_b = A.alloc("w_out_b", [128, 8, D], BF16)
    kT = [A.alloc("kT%d" % h, [128, T], BF16) for h in range(4)]
    vaug = A.alloc("vaug", [128, NKB, 4, 130], BF16)
    lbo = A.alloc("lbo", [128, 8], F32)
    gnw_b = A.alloc("gnw_b", [128, 128], F32)
    sub_b = A.alloc("sub_b", [128, 128], F32)
    lam4 = A.alloc("lam4", [128, 4, 64], F32)
    lamv = A.alloc("lamv", [128, 4], F32)
    neglam = A.alloc("neglam", [128, 1], F32)
    abias = A.alloc("abias", [128, 4, 16], F32)
    iot = A.alloc("iot", [128, 16], F32)
    xt = A.alloc("xt", [128, D], F32)
    xn = A.alloc("xn", [128, D], F32)
    uT = A.alloc("uT", [128, 8, TB], BF16)
    qTa = [A.alloc("qTa%d" % h, [128, TB], BF16) for h in range(4)]
    t_sig = A.alloc("t_sig", [128, TB], F32)
    t_lf = A.alloc("t_lf", [128, TB], F32)
    t_omf = A.alloc("t_omf", [128, TB], F32)
    t_b = A.alloc("t_b", [128, TB], F32)
    t_E = A.alloc("t_E", [128, TB], F32)
    t_Ei = A.alloc("t_Ei", [128, TB], F32)
    scanmask = A.alloc("scanmask", [128, TB], F32)
    qtil = A.alloc("qtil", [128, TB], BF16)
    ktilT = A.alloc("ktilT", [128, TB], BF16)
    ktil_tok = A.alloc("ktil_tok", [128, NSUB, 128], BF16)
    erbl = A.alloc("erbl", [128, NCH, 2], F32)
    v_tok = A.alloc("v_tok", [128, NSUB, 512], BF16)
    sg_tok = A.alloc("sg_tok", [128, NSUB, 512], BF16)
    sgtmp = A.alloc("sgtmp", [128, 512], F32)
    S = A.alloc("S", [128, 4, 128], F32)
    Sr = A.alloc("Sr", [128, 4, 128], BF16)
    kvtmp = A.alloc("kvtmp", [128, 128], F32)
    AT_sb = A.alloc("AT_sb", [128, 64], BF16)
    PT = [A.alloc("PT%d" % i, [128, 512], BF16) for i in range(2)]
    t0 = A.alloc("t0", [128, 128], F32)
    o_f = A.alloc("o_f", [128, 128], F32)
    o_b = A.alloc("o_b", [128, 128], BF16)
    junk = A.alloc("junk", [128, 128], F32)
    rl = A.alloc("rl", [128, 2], F32)
    mixT = A.alloc("mixT", [128, 8, TB], BF16)
    h1s = xt
    wada_t = [nc.alloc_sbuf_tensor_at("wadaA%d" % i, [128, 8, 512], BF16, offset=A.off + i * 8192) for i in range(2)]
    assert A.off + 16384 <= A.limit, A.off

    ada_part([0, 1], 2, wada_t)
    make_scl(0, 1, O_N1)
    for cc in range(INW // 512):
        DMA(w_in_b[:, :, cc * 512:(cc + 1) * 512], w_in[:, cc * 512:(cc + 1) * 512].rearrange("(k p) n -> p k n", p=128),
            [], ["w_in_b"], eng="pool")
    for cc in range(2):
        DMA(w_out_b[:, :, cc * 512:(cc + 1) * 512], w_out[:, cc * 512:(cc + 1) * 512].rearrange("(k p) n -> p k n", p=128),
            [], ["w_out_b"], eng="pool")
    l0 = stgT[:, O_LB:O_LB + 4]
    l1 = stgT[:, O_LB + 4:O_LB + 8]
    TT("dve", lbo[:, 4:8], l1, l0, ALU.subtract, ["stgT"], ["lbo"])
    ACT(lbo[:, 4:8], lbo[:, 4:8], AF.Exp, ["lbo"], ["lbo"])
    TS("dve", lbo[:, 4:8], lbo[:, 4:8], 1.0, None, ALU.add, None, ["lbo"], ["lbo"])
    RECIP(lbo[:, 0:4], lbo[:, 4:8], ["lbo"], ["lbo"])
    TS("dve", lbo[:, 4:8], lbo[:, 0:4], -1.0, 1.0, ALU.mult, ALU.add, ["lbo"], ["lbo"])
    DMA(gnw_b[:], gnw.partition_broadcast(128), [], ["gnw_b"])
    DMA(sub_b[:], subw.partition_broadcast(128), [], ["sub_b"])
    TS("dve", sub_b[:], sub_b[:], 0.8, None, ALU.mult, None, ["sub_b"], ["sub_b"])
    for i, v in enumerate((lq1, lk1, lq2, lk2)):
        DMA(lam4[:, i, :], v.partition_broadcast(128), [], ["lam4"])
    TT("dve", lam4[:, 0, :], lam4[:, 0, :], lam4[:, 1, :], ALU.mult, ["lam4"], ["lam4"])
    TT("dve", lam4[:, 2, :], lam4[:, 2, :], lam4[:, 3, :], ALU.mult, ["lam4"], ["lam4"])
    P.op("dve", lambda e: e.reduce_sum(out=lamv[:, 0:1], in_=lam4[:, 0, :], axis=AX.X), ["lam4"], ["lamv"])
    P.op("dve", lambda e: e.reduce_sum(out=lamv[:, 1:2], in_=lam4[:, 2, :], axis=AX.X), ["lam4"], ["lamv"])
    ACT(lamv[:, 0:2], lamv[:, 0:2], AF.Exp, ["lamv"], ["lamv"])
    TT("dve", neglam[:], lamv[:, 1:2], lamv[:, 0:1], ALU.subtract, ["lamv"], ["neglam"])
    TS("dve", neglam[:], neglam[:], -0.2, None, ALU.add, None, ["neglam"], ["neglam"])
    P.op("pool", lambda e: e.iota(iot[:], pattern=[[128, 16]], base=0, channel_multiplier=1, allow_small_or_imprecise_dtypes=True), [], ["iot"])
    W_H = [128, min(512, TB), min(512, TB), min(512, TB)]
    OFF_H = [15, 12, 12, 12]
    for h in range(4):
        slope = 2.0 ** (-2.0 * (h + 1))
        TS("dve", abias[:, h, :], iot[:], -128.0 * OFF_H[h], slope, ALU.add, ALU.mult, ["iot"], ["abias"])
    MEMSET("pool", vaug[:, :, :, 128:130], 1.0, ["vaug"])
    MEMSET("pool", scanmask[:], 1.0, ["scanmask"])
    MEMSET("pool", scanmask[:].rearrange("p (c t) -> p c t", t=64)[:, :, 0:1], 0.0, ["scanmask"])
    P.fence()

    HQ, HF, HI, HG, AQ, AK, AV = 0, 512, 1024, 1536, 2048, 2560, 3072
    pb_rr = [0]

    def next_pb():
        pb_rr[0] ^= 1
        return pb_rr[0]

    def proj_feat(col0, consume):
        b = next_pb()
        for k in range(8):
            MM(pbank(b)[:, 0:TB], w_in_b[:, k, col0:col0 + 128], uT[:, k, :], k == 0, k == 7, ["w_in_b", "uT"], ["pb%d" % b])
        consume(pbank(b)[:, 0:TB], "pb%d" % b)

    def proj_tok(col0, sub, consume):
        b = next_pb()
        for k in range(8):
            MM(pbank(b)[:], uT[:, k, sub * 128:(sub + 1) * 128], w_in_b[:, k, col0:col0 + 512], k == 0, k == 7, ["w_in_b", "uT"], ["pb%d" % b])
        consume(pbank(b)[:], "pb%d" % b)

    for s in range(NSEQ):
        MEMSET("pool", S[:], 0.0, ["S"])
        for blk in range(NBLK):
            tok0 = s * T + blk * TB
            for sub in range(NSUB):
                DMA(xt[:], x[tok0 + sub * 128: tok0 + (sub + 1) * 128, :], [], ["xt"])
                norm_to_uT(xt[:], s, 0, 0, uT, sub * 128, xn, "xt")
            for h in range(4):
                proj_feat(AQ + h * 128, lambda ps, key, h=h: CP("act", qTa[h][:], ps, [key], ["qTa%d" % h]))
                proj_feat(AK + h * 128, lambda ps, key, h=h: CP("dve", kT[h][:, blk * TB:(blk + 1) * TB], ps, [key], ["kT%d" % h]))
            for sub in range(NSUB):
                kb = blk * NSUB + sub
                proj_tok(AV, sub, lambda ps, key, kb=kb: CP("act", vaug[:, kb, :, 0:128], ps.rearrange("p (h v) -> p h v", v=128), [key], ["vaug"]))
            for h in range(4):
                W = W_H[h]
                nqt = TB // W
                qpt = W // 128
                last_kb = blk * NSUB + NSUB - 1
                acc_started = set()
                for kb in range(last_kb + 1):
                    for m in range(2):
                        for qt in range(nqt):
                            gq0 = blk * TB + qt * W
                            if kb * 128 >= gq0 + W:
                                continue
                            lo = max(gq0, kb * 128)
                            nq = gq0 + W - lo
                            lq = lo - blk * TB
                            d = kb - gq0 // 128 + OFF_H[h]
                            sb_ = 2 + ((kb + m + qt) % 2)
                            pt = PT[(kb + m + qt) % 2]
                            ptk = "PT%d" % ((kb + m + qt) % 2)
                            MM(pbank(sb_)[:, 0:nq], kT[h][m * 64:(m + 1) * 64, kb * 128:(kb + 1) * 128],
                               qTa[h][m * 64:(m + 1) * 64, lq:lq + nq], True, True, ["kT%d" % h, "qTa%d" % h], ["pb%d" % sb_])
                            ACT(pt[:, 0:nq], pbank(sb_)[:, 0:nq], AF.Exp, ["pb%d" % sb_, "abias"], [ptk], scale=0.125, bias=abias[:, h, d:d + 1])
                            if lo == kb * 128:
                                TT("pool", pt[:, 0:128], pt[:, 0:128], mask_b[:], ALU.mult, [ptk, "mask_b"], [ptk])
                            for j in range(nq // 128):
                                qb = (lq // 128) + j
                                a = m * NSUB + qb
                                ab, asl = 5 + a // 3, (a % 3) * 130
                                first = ab not in acc_started
                                acc_started.add(ab)
                                MM(pbank(ab)[:, asl:asl + 130], pt[:, j * 128:(j + 1) * 128], vaug[:, kb, h, :], first,
                                   kb == blk * NSUB + qb, [ptk, "vaug"], ["pb%d" % ab], skip_group_check=True)
                for qb in range(NSUB):
                    a0, a1 = qb, NSUB + qb
                    acc0 = pbank(5 + a0 // 3)[:, (a0 % 3) * 130:(a0 % 3) * 130 + 130]
                    acc1 = pbank(5 + a1 // 3)[:, (a1 % 3) * 130:(a1 % 3) * 130 + 130]
                    k0, k1 = "pb%d" % (5 + a0 // 3), "pb%d" % (5 + a1 // 3)
                    RECIP(rl[:, 0:1], acc0[:, 128:129], [k0], ["rl"])
                    RECIP(rl[:, 1:2], acc1[:, 128:129], [k1], ["rl"])
                    TT("dve", rl[:, 1:2], rl[:, 1:2], neglam[:], ALU.mult, ["rl", "neglam"], ["rl"])
                    TS("dve", t0[:], acc0[:, 0:128], rl[:, 0:1], None, ALU.mult, None, [k0, "rl"], ["t0"])
                    STT("dve", o_f[:], acc1[:, 0:128], rl[:, 1:2], t0[:], ALU.mult, ALU.add, [k1, "rl", "t0"], ["o_f"])
                    ACT(junk[:], o_f[:], AF.Square, ["o_f"], ["junk", "ssq"], accum_out=ssq[:, 1:2])
                    rstd_from_ssq(ssq[:, 1:2], rstd[:, 1:2], 128, "")
                    STT("dve", o_b[:], o_f[:], rstd[:, 1:2], sub_b[:], ALU.mult, ALU.mult, ["o_f", "rstd", "sub_b"], ["o_b"])
                    TR(pbank(4)[:].bitcast(BF16)[:, 0:128], o_b[:], ident_b[:], ["o_b", "ident_b"], ["pb4"])
                    CP("act", mixT[:, 4 + h, qb * 128:(qb + 1) * 128], pbank(4)[:].bitcast(BF16)[:, 0:128], ["pb4"], ["mixT"])
            for sub in range(NSUB):
                proj_tok(HI, sub, lambda ps, key, sub=sub: CP("act", v_tok[:, sub, :], ps, [key], ["v_tok"]))

                def gate(ps, key, sub=sub):
                    ACT(sgtmp[:], ps, AF.Exp, [key], ["sgtmp"], scale=-1.0)
                    TS("dve", sgtmp[:], sgtmp[:], 1.0, None, ALU.add, None, ["sgtmp"], ["sgtmp"])
                    RECIP(sgtmp[:], sgtmp[:], ["sgtmp"], ["sgtmp"])
                    TT("dve", sg_tok[:, sub, :], ps, sgtmp[:], ALU.mult, [key, "sgtmp"], ["sg_tok"])
                proj_tok(HG, sub, gate)
            for h in range(4):
                def fchain(ps, key, h=h):
                    ACT(t_sig[:], ps, AF.Exp, [key], ["t_sig"], scale=-1.0)
                    TS("dve", t_sig[:], t_sig[:], 1.0, None, ALU.add, None, ["t_sig"], ["t_sig"])
                    RECIP(t_sig[:], t_sig[:], ["t_sig"], ["t_sig"])
                    TS("dve", t_sig[:], t_sig[:], lbo[:, 4 + h:5 + h], lbo[:, h:h + 1], ALU.mult, ALU.add, ["t_sig", "lbo"], ["t_sig"])
                    ACT(t_lf[:], t_sig[:], AF.Ln, ["t_sig"], ["t_lf"])
                    TS("pool", t_omf[:], t_sig[:], -1.0, 1.0, ALU.mult, ALU.add, ["t_sig"], ["t_omf"])
                    P.op("dve", lambda e: e.tensor_tensor_scan(out=t_b[:], data0=scanmask[:], data1=t_lf[:], initial=0.0,
                                                               op0=ALU.mult, op1=ALU.add), ["scanmask", "t_lf"], ["t_b"])
                    b3 = t_b[:].rearrange("p (c t) -> p c t", t=64)
                    ACT(erbl[:], b3[:, :, 31:64:32], AF.Exp, ["t_b"], ["erbl"])
                    TT("dve", t_lf[:].rearrange("p (c t) -> p c t", t=64), b3, b3[:, :, 31:32].to_broadcast([128, NCH, 64]), ALU.subtract,
                       ["t_b"], ["t_lf"])
                    ACT(t_E[:], t_lf[:], AF.Exp, ["t_lf"], ["t_E"])
                    ACT(t_Ei[:], t_lf[:], AF.Exp, ["t_lf"], ["t_Ei"], scale=-1.0)
                    TT("pool", ktilT[:], t_omf[:], t_Ei[:], ALU.mult, ["t_omf", "t_Ei"], ["ktilT"])
                proj_feat(HF + h * 128, fchain)
                proj_feat(HQ + h * 128, lambda ps, key: TT("dve", qtil[:], ps, t_E[:], ALU.mult, [key, "t_E"], ["qtil"]))
                for sub in range(NSUB):
                    TR(pbank(4)[:].bitcast(BF16)[:, 0:128], ktilT[:, sub * 128:(sub + 1) * 128], ident_b[:], ["ktilT", "ident_b"], ["pb4"])
                    CP("act", ktil_tok[:, sub, :], pbank(4)[:].bitcast(BF16)[:, 0:128], ["pb4"], ["ktil_tok"])
                for sub in range(NSUB):
                    for half in range(2):
                        c = sub * 2 + half
                        r0 = half * 64
                        cs = slice(c * 64, (c + 1) * 64)
                        TS("dve", Sr[:, h, :], S[:, h, :], erbl[:, c, 0:1], None, ALU.mult, None, ["S", "erbl"], ["Sr"])
                        MM(pbank(3)[r0:r0 + 64, 0:128], qtil[:, cs], Sr[:, h, :], True, False, ["qtil", "Sr"], ["pb3"], tile_position=(0, r0))
                        MM(pbank(2)[r0:r0 + 64, 0:64], ktilT[:, cs], qtil[:, cs], True, True, ["ktilT", "qtil"], ["pb2"], tile_position=(0, r0))
                        TT("dve", AT_sb[r0:r0 + 64, :], pbank(2)[r0:r0 + 64, 0:64], mask_f[r0:r0 + 64, r0:r0 + 64], ALU.mult, ["pb2", "mask_f"], ["AT_sb"])
                        MM(pbank(3)[r0:r0 + 64, 0:128], AT_sb[r0:r0 + 64, :], v_tok[r0:r0 + 64, sub, h * 128:(h + 1) * 128], False, True,
                           ["AT_sb", "v_tok"], ["pb3"], tile_position=(r0, r0))
                        MM(pbank(2)[:, 128:256], ktil_tok[r0:r0 + 64, sub, :], v_tok[r0:r0 + 64, sub, h * 128:(h + 1) * 128], True, True,
                           ["ktil_tok", "v_tok"], ["pb2"])
                        TS("dve", kvtmp[:], pbank(2)[:, 128:256], t_E[:, c * 64 + 63:c * 64 + 64], None, ALU.mult, None, ["pb2", "t_E"], ["kvtmp"])
                        STT("dve", S[:, h, :], S[:, h, :], erbl[:, c, 1:2], kvtmp[:], ALU.mult, ALU.add, ["S", "erbl", "kvtmp"], ["S"])
                    ACT(junk[:], pbank(3)[:, 0:128], AF.Square, ["pb3"], ["junk", "ssq"], accum_out=ssq[:, 2:3])
                    rstd_from_ssq(ssq[:, 2:3], rstd[:, 2:3], 128, "")
                    STT("dve", o_f[:], pbank(3)[:, 0:128], rstd[:, 2:3], gnw_b[:], ALU.mult, ALU.mult, ["pb3", "rstd", "gnw_b"], ["o_f"])
                    TT("dve", o_b[:], o_f[:], sg_tok[:, sub, h * 128:(h + 1) * 128], ALU.mult, ["o_f", "sg_tok"], ["o_b"])
                    TR(pbank(4)[:].bitcast(BF16)[:, 0:128], o_b[:], ident_b[:], ["o_b", "ident_b"], ["pb4"])
                    CP("act", mixT[:, h, sub * 128:(sub + 1) * 128], pbank(4)[:].bitcast(BF16)[:, 0:128], ["pb4"], ["mixT"])
            for sub in range(NSUB):
                DMA(h1s[:], x[tok0 + sub * 128: tok0 + (sub + 1) * 128, :], [], ["xt"])
                for half in range(2):
                    b = 5 + half
                    for k in range(8):
                        MM(pbank(b)[:], mixT[:, k, sub * 128:(sub + 1) * 128], w_out_b[:, k, half * 512:(half + 1) * 512], k == 0, k == 7,
                           ["mixT", "w_out_b"], ["pb%d" % b])
                    TT("dve", xn[:, half * 512:(half + 1) * 512], pbank(b)[:], gb[s][:, half * 512:(half + 1) * 512], ALU.mult,
                       ["pb%d" % b, "gb%d" % s], ["xn"])
                TT("pool", h1s[:], h1s[:], xn[:], ALU.add, ["xt", "xn"], ["xt"])
                DMA(h1[tok0 + sub * 128: tok0 + (sub + 1) * 128, :], h1s[:], ["xt"], ["h1dram"])
    P.fence()

    NSB = TBB // 128
    A.off = phase_base
    w_g_b = A.alloc("w_g_b", [128, 8, DFF], BF16)
    w_u_b = A.alloc("w_u_b", [128, 8, DFF], BF16)
    w_d_b = A.alloc("w_d_b", [128, NKF, D], BF16)
    fnw_b = A.alloc("fnw_b", [128, D], F32)
    ht = [A.alloc("ht%d" % i, [128, D], F32) for i in range(NSB)]
    xn2 = A.alloc("xn2", [128, D], F32)
    u2T = A.alloc("u2T", [128, 8, TBB], BF16)
    hidT = A.alloc("hidT", [128, NKF, TBB], BF16)
    eg = A.alloc("eg", [128, TBB], F32)
    gg = A.alloc("gg", [128, TBB], F32)
    h2 = A.alloc("h2", [128, D], F32)
    outs = xn2
    wada_tb = [nc.alloc_sbuf_tensor_at("wadaB%d" % i, [128, 8, 512], BF16, offset=A.off + i * 8192) for i in range(2)]
    assert A.off + 16384 <= A.limit, A.off

    ada_part([3, 4], 5, wada_tb)
    make_scl(1, 4, O_N2)
    for cc in range(0, DFF, 512):
        w = min(512, DFF - cc)
        DMA(w_g_b[:, :, cc:cc + w], w_g[:, cc:cc + w].rearrange("(k p) n -> p k n", p=128), [], ["w_g_b"], eng="pool")
        DMA(w_u_b[:, :, cc:cc + w], w_u[:, cc:cc + w].rearrange("(k p) n -> p k n", p=128), [], ["w_u_b"], eng="pool")
    for k2 in range(0, NKF, 2):
        DMA(w_d_b[:, k2:k2 + 2, :], w_d[k2 * 128:(k2 + 2) * 128, :].rearrange("(k p) n -> p k n", p=128), [], ["w_d_b"], eng="pool")
    DMA(fnw_b[:], fnw.partition_broadcast(128), [], ["fnw_b"])

    for s in range(NSEQ):
        for blk in range(T // TBB):
            tok0 = s * T + blk * TBB
            for sub in range(NSB):
                DMA(ht[sub][:], h1[tok0 + sub * 128: tok0 + (sub + 1) * 128, :], ["h1dram"], ["ht%d" % sub])
                norm_to_uT(ht[sub][:], s, 1, 3, u2T, sub * 128, xn2, "ht%d" % sub)
            for f in range(NKF):
                bg, bu = 0, 1
                for k in range(8):
                    MM(pbank(bg)[:, 0:TBB], w_g_b[:, k, f * 128:(f + 1) * 128], u2T[:, k, :], k == 0, k == 7, ["w_g_b", "uT"], ["pb0"])
                for k in range(8):
                    MM(pbank(bu)[:, 0:TBB], w_u_b[:, k, f * 128:(f + 1) * 128], u2T[:, k, :], k == 0, k == 7, ["w_u_b", "uT"], ["pb1"])
                ACT(eg[:], pbank(bg)[:, 0:TBB], AF.Exp, ["pb0"], ["eg"], scale=-1.0)
                TS("dve", eg[:], eg[:], 1.0, None, ALU.add, None, ["eg"], ["eg"])
                RECIP(eg[:], eg[:], ["eg"], ["eg"])
                TT("dve", gg[:], pbank(bg)[:, 0:TBB], eg[:], ALU.mult, ["pb0", "eg"], ["gg"])
                TT("dve", hidT[:, f, :], pbank(bu)[:, 0:TBB], gg[:], ALU.mult, ["pb1", "gg"], ["hidT"])
            for sub in range(NSB):
                for half in range(2):
                    b = 5 + half
                    for f in range(NKF):
                        MM(pbank(b)[:], hidT[:, f, sub * 128:(sub + 1) * 128], w_d_b[:, f, half * 512:(half + 1) * 512], f == 0, f == NKF - 1,
                           ["hidT", "w_d_b"], ["pb%d" % b])
                    TT("dve", h2[:, half * 512:(half + 1) * 512], pbank(b)[:], gb[s][:, half * 512:(half + 1) * 512], ALU.mult,
                       ["pb%d" % b, "gb%d" % s], ["h2"])
                TT("pool", h2[:], h2[:], ht[sub][:], ALU.add, ["h2", "ht%d" % sub], ["h2"])
                ACT(outs[:], h2[:], AF.Square, ["h2"], ["xn", "ssq"], accum_out=ssq[:, 3:4])
                rstd_from_ssq(ssq[:, 3:4], rstd[:, 3:4], D, "")
                STT("dve", outs[:], h2[:], rstd[:, 3:4], fnw_b[:], ALU.mult, ALU.mult, ["h2", "rstd", "fnw_b"], ["xn"])
                DMA(out[tok0 + sub * 128: tok0 + (sub + 1) * 128, :], outs[:], ["xn"], ["outdram"])

    P.analyze()
    esems = {e: nc.alloc_semaphore("es_" + e) for e in ENGS}
    dsems = [nc.alloc_semaphore("ds%d" % i) for i in range(P.n_dma_sems)]
    with nc.Block() as block:
        P.emit(block, esems, dsems)
    return nc


def core_inputs(inputs, core, NSEQ, T):
    f = lambda a: np.ascontiguousarray(np.asarray(a, dtype=np.float32))
    sl = slice(core * NSEQ, (core + 1) * NSEQ)
    return {
        "x": f(np.asarray(inputs["x"])[sl].reshape(NSEQ * T, D)),
        "c": f(np.asarray(inputs["c"])[sl].reshape(NSEQ * 8, 128)),
        "w_ada": f(np.asarray(inputs["w_ada"])[0]),
        "b_ada": f(np.asarray(inputs["b_ada"])[0]),
        "norm1_w": f(np.asarray(inputs["norm1_w"])[0].reshape(8, 128)),
        "w_in": f(np.asarray(inputs["w_in"])[0]),
        "lb_logits": f(np.asarray(inputs["hgrn_lb_logits"]).reshape(8, 128)),
        "gnorm_w": f(np.asarray(inputs["hgrn_gnorm_w"])[0]),
        "lq1": f(np.asarray(inputs["diff_lambda_q1"])[0]),
        "lk1": f(np.asarray(inputs["diff_lambda_k1"])[0]),
        "lq2": f(np.asarray(inputs["diff_lambda_q2"])[0]),
        "lk2": f(np.asarray(inputs["diff_lambda_k2"])[0]),
        "subln_w": f(np.asarray(inputs["diff_subln_w"])[0]),
        "w_out": f(np.asarray(inputs["w_out"])[0]),
        "norm2_w": f(np.asarray(inputs["norm2_w"])[0].reshape(8, 128)),
        "w_g": f(np.asarray(inputs["w_ffn_gate"])[0]),
        "w_u": f(np.asarray(inputs["w_ffn_up"])[0]),
        "w_d": f(np.asarray(inputs["w_ffn_down"])[0]),
        "fnw": f(np.asarray(inputs["final_norm_w"])),
    }


def kernel(**inputs):
    B, T, _ = np.asarray(inputs["x"]).shape
    n = 8
    NSEQ = B // n
    nc = build(T=T, NSEQ=NSEQ)
    in_maps = [core_inputs(inputs, i, NSEQ, T) for i in range(n)]
    res = run_bass_kernel_spmd(nc, in_maps, core_ids=list(range(n)))
    outs = [np.asarray(r["out"]).reshape(NSEQ, T, D) for r in res.results]
    return np.concatenate(outs, axis=0).astype(np.float32)
```

```python
import numpy as np
import os as _os2
import concourse.bass as bass
import concourse.mybir as mybir
from concourse.bass_utils import run_bass_kernel_spmd

F32 = mybir.dt.float32
BF16 = mybir.dt.bfloat16
AF = mybir.ActivationFunctionType
ALU = mybir.AluOpType
AX = mybir.AxisListType

D = 1024
DFF = 2816
NKF = DFF // 128
INW = 3584
ENGS = ("pe", "act", "dve", "pool", "sp")


class Op:
    __slots__ = ("eng", "fn", "reads", "writes", "dma", "waits", "mark", "seq", "dsem", "dval", "idx", "fence")

    def __init__(self, eng, fn, reads, writes, dma):
        self.eng = eng
        self.fn = fn
        self.reads = tuple(reads)
        self.writes = tuple(writes)
        self.dma = dma
        self.waits = []
        self.mark = False
        self.seq = 0
        self.dsem = None
        self.dval = 0
        self.fence = False


class Prog:
    def __init__(self, n_dma_sems=24):
        self.ops = []
        self.n_dma_sems = n_dma_sems

    def op(self, eng, fn, reads=(), writes=(), dma=False):
        o = Op(eng, fn, reads, writes, dma)
        o.idx = len(self.ops)
        self.ops.append(o)
        return o

    def fence(self):
        o = Op("sp", None, (), (), False)
        o.idx = len(self.ops)
        o.fence = True
        self.ops.append(o)

    def analyze(self):
        last_writer = {}
        readers = {}
        need = []
        last_eng = {e: None for e in ENGS}
        pend_dma = []
        fence_deps = []
        for o in self.ops:
            if o.fence:
                fence_deps = [j for j in last_eng.values() if j is not None] + list(pend_dma)
                pend_dma = []
                last_writer = {}
                readers = {}
                need.append([])
                continue
            deps = {}
            for j in fence_deps:
                deps[j] = "fence"
            for r in o.reads:
                j = last_writer.get(r)
                if j is not None:
                    deps[j] = "raw"
            for w in o.writes:
                j = last_writer.get(w)
                if j is not None and j not in deps:
                    deps[j] = "waw"
                for j in readers.get(w, ()):
                    if j not in deps and j != o.idx:
                        deps[j] = "war"
            for r in o.reads:
                readers.setdefault(r, []).append(o.idx)
            for w in o.writes:
                last_writer[w] = o.idx
                readers[w] = []
            nd = []
            for j, kind in deps.items():
                p = self.ops[j]
                if p.dma:
                    nd.append(j)
                elif p.eng == o.eng:
                    if o.dma:
                        p.mark = True
                        nd.append(j)
                    elif p.eng == "pe":
                        continue
                    elif kind != "fence":
                        p.mark = True
                        nd.append(j)
                else:
                    p.mark = True
                    nd.append(j)
            need.append(nd)
            if o.dma:
                pend_dma.append(o.idx)
            else:
                last_eng[o.eng] = o.idx
        for e, j in last_eng.items():
            if j is not None:
                self.ops[j].mark = True
        cnt = {e: 0 for e in ENGS}
        dcnt = [0] * self.n_dma_sems
        k = 0
        kp = 0
        for o in self.ops:
            if o.fence:
                continue
            if o.dma:
                if o.eng == "pool":
                    o.dsem = kp % 8
                    kp += 1
                else:
                    o.dsem = 8 + k % (self.n_dma_sems - 8)
                    k += 1
                dcnt[o.dsem] += 1
                o.dval = 16 * dcnt[o.dsem]
            elif o.mark:
                cnt[o.eng] += 1
                o.seq = cnt[o.eng]
        waited = {e: {} for e in ENGS}
        for o in self.ops:
            if o.fence:
                continue
            w = {}
            for j in need[o.idx]:
                p = self.ops[j]
                if p.dma:
                    key, val = ("d", p.dsem), p.dval
                else:
                    key, val = ("e", p.eng), p.seq
                if val > w.get(key, 0):
                    w[key] = val
            if o.dma and o.dval > 16:
                key = ("d", o.dsem)
                w[key] = max(w.get(key, 0), o.dval - 16)
            ws = waited[o.eng]
            for key, val in w.items():
                if ws.get(key, 0) < val:
                    ws[key] = val
                    o.waits.append((key, val))
        self.final_dma = [(i, 16 * c) for i, c in enumerate(dcnt) if c > 0]
        self.final_cnt = cnt

    def emit(self, block, esems, dsems):
        def run_engine(ename, eng):
            for o in self.ops:
                if o.fence or o.eng != ename:
                    continue
                for key, val in o.waits:
                    sem = dsems[key[1]] if key[0] == "d" else esems[key[1]]
                    eng.wait_ge(sem, val)
                ins = o.fn(eng)
                if o.dma:
                    ins.then_inc(dsems[o.dsem], 16)
                elif o.mark:
                    ins.then_inc(esems[o.eng], 1)
            if ename == "sp":
                for i, v in self.final_dma:
                    eng.wait_ge(dsems[i], v)
                for e in ENGS:
                    if e != "sp" and self.final_cnt[e] > 0:
                        eng.wait_ge(esems[e], self.final_cnt[e])

        block.tensor(lambda e: run_engine("pe", e))
        block.scalar(lambda e: run_engine("act", e))
        block.vector(lambda e: run_engine("dve", e))
        block.gpsimd(lambda e: run_engine("pool", e))
        block.sync(lambda e: run_engine("sp", e))


class Arena:
    def __init__(self, nc, base=16640, limit=229376):
        self.nc = nc
        self.off = base
        self.limit = limit
        self.n = 0

    def alloc(self, name, shape, dt):
        isz = 2 if dt == BF16 else 4
        size = isz
        for s in shape[1:]:
            size *= s
        size = (size + 63) // 64 * 64
        off = self.off
        assert off + size <= self.limit, ("SBUF overflow", name, off, size)
        self.off += size
        self.n += 1
        return self.nc.alloc_sbuf_tensor_at(name, list(shape), dt, offset=off)


def build(T=2048, NSEQ=2, TB=512, TBB=512):
    NSUB = TB // 128
    NBLK = T // TB
    NCH = TB // 64
    NKB = T // 128
    NTOK = NSEQ * T
    nc = bass.Bass("TRN2", target_bir_lowering=False)

    def din(name, shape):
        return nc.dram_tensor(name, list(shape), F32, kind="ExternalInput").ap()

    x = din("x", [NTOK, D])
    c_in = din("c", [NSEQ * 8, 128])
    w_ada = din("w_ada", [D, 6 * D])
    b_ada = din("b_ada", [6 * D])
    n1w = din("norm1_w", [8, 128])
    w_in = din("w_in", [D, INW])
    lbl = din("lb_logits", [8, 128])
    gnw = din("gnorm_w", [128])
    lq1 = din("lq1", [64])
    lk1 = din("lk1", [64])
    lq2 = din("lq2", [64])
    lk2 = din("lk2", [64])
    subw = din("subln_w", [128])
    w_out = din("w_out", [D, D])
    n2w = din("norm2_w", [8, 128])
    w_g = din("w_g", [D, DFF])
    w_u = din("w_u", [D, DFF])
    w_d = din("w_d", [DFF, D])
    fnw = din("fnw", [D])
    out = nc.dram_tensor("out", [NTOK, D], F32, kind="ExternalOutput").ap()
    h1 = nc.dram_tensor("h1_scratch", [NTOK, D], F32, kind="Internal").ap()

    P = Prog()

    def MARK(name):
        print('MARK', name, len(P.ops))
    A = Arena(nc)

    pbs = [nc.alloc_psum_tensor("pb%d" % i, [128, 512], F32) for i in range(8)]

    def pbank(i):
        return pbs[i][:]

    def MM(out_, lhsT, rhs, start, stop, reads, writes, **kw):
        P.op("pe", lambda e: e.matmul(out_, lhsT=lhsT, rhs=rhs, start=start, stop=stop, **kw), reads, writes)

    def TR(out_, in_, ident, reads, writes):
        P.op("pe", lambda e: e.transpose(out=out_, in_=in_, identity=ident), reads, writes)

    def ACT(out_, in_, func, reads, writes, bias=0.0, scale=1.0, accum_out=None):
        if accum_out is None:
            P.op("act", lambda e: e.activation(out=out_, in_=in_, func=func, bias=bias, scale=scale), reads, writes)
        else:
            P.op("act", lambda e: e.activation(out=out_, in_=in_, func=func, bias=bias, scale=scale, accum_out=accum_out), reads, writes)

    def TS(eng, out_, in0, s1, s2, op0, op1, reads, writes):
        if s2 is None:
            P.op(eng, lambda e: e.tensor_scalar(out=out_, in0=in0, scalar1=s1, scalar2=None, op0=op0), reads, writes)
        else:
            P.op(eng, lambda e: e.tensor_scalar(out=out_, in0=in0, scalar1=s1, scalar2=s2, op0=op0, op1=op1), reads, writes)

    def TT(eng, out_, in0, in1, op, reads, writes):
        P.op(eng, lambda e: e.tensor_tensor(out=out_, in0=in0, in1=in1, op=op), reads, writes)

    def STT(eng, out_, in0, scalar, in1, op0, op1, reads, writes):
        P.op(eng, lambda e: e.scalar_tensor_tensor(out=out_, in0=in0, scalar=scalar, in1=in1, op0=op0, op1=op1), reads, writes)

    def CP(eng, out_, in_, reads, writes):
        if eng == "act":
            P.op("act", lambda e: e.copy(out=out_, in_=in_), reads, writes)
        else:
            P.op(eng, lambda e: e.tensor_copy(out=out_, in_=in_), reads, writes)

    def RECIP(out_, in_, reads, writes):
        P.op("dve", lambda e: e.reciprocal(out=out_, in_=in_), reads, writes)

    def MEMSET(eng, ap, val, writes):
        P.op(eng, lambda e: e.memset(ap, val), (), writes)

    def DMA(out_, in_, reads, writes, eng="sp", **kw):
        P.op(eng, lambda e: e.dma_start(out=out_, in_=in_, **kw), reads, writes, dma=True)

    def rstd_from_ssq(ssq, rstd, n, key):
        TS("dve", rstd, ssq, 1.0 / n, 1e-6, ALU.mult, ALU.add, [key + "ssq"], [key + "rstd"])
        ACT(rstd, rstd, AF.Ln, [key + "rstd"], [key + "rstd"])
        ACT(rstd, rstd, AF.Exp, [key + "rstd"], [key + "rstd"], scale=-0.5)

    ident_f = A.alloc("ident_f", [128, 128], F32)
    ident_b = A.alloc("ident_b", [128, 128], BF16)
    ones_f = A.alloc("ones_f", [128, 128], F32)
    mask_f = A.alloc("mask_f", [128, 128], F32)
    mask_b = A.alloc("mask_b", [128, 128], BF16)
    NST = 8 * NSEQ + 8 + 8 + 48 + 8
    O_C, O_N1, O_N2, O_BA, O_LB = 0, 8 * NSEQ, 8 * NSEQ + 8, 8 * NSEQ + 16, 8 * NSEQ + 64
    stg = A.alloc("stg", [128, 128], F32)
    stgT = A.alloc("stgT", [128, NST], F32)
    modT = A.alloc("modT", [128, 6, 8, NSEQ], F32)
    scl = A.alloc("scl", [128, 2, 8, NSEQ], F32)
    silf = A.alloc("silf", [128, NSEQ * 8], F32)
    siltmp = A.alloc("siltmp", [128, NSEQ * 8], F32)
    scT_b = A.alloc("scT_b", [128, 8, NSEQ], BF16)
    ssq = A.alloc("ssq", [128, 8], F32)
    rstd = A.alloc("rstd", [128, 8], F32)
    phase_base = A.off

    MEMSET("pool", ident_f[:], 0.0, ["ident_f"])
    P.op("pool", lambda e: e.affine_select(out=ident_f[:], in_=ident_f[:], pattern=[[-1, 128]], compare_op=ALU.not_equal,
                                           fill=1.0, base=0, channel_multiplier=1), ["ident_f"], ["ident_f"])
    CP("pool", ident_b[:], ident_f[:], ["ident_f"], ["ident_b"])
    MEMSET("pool", ones_f[:], 1.0, ["ones_f"])
    P.op("pool", lambda e: e.affine_select(out=mask_f[:], in_=ones_f[:], pattern=[[1, 128]], compare_op=ALU.is_ge,
                                           fill=0.0, base=0, channel_multiplier=-1), ["ones_f"], ["mask_f"])
    CP("pool", mask_b[:], mask_f[:], ["mask_f"], ["mask_b"])
    MEMSET("dve", stg[:], 0.0, ["stg"])
    DMA(stg[O_C:O_C + 8 * NSEQ, :], c_in[:, :], [], ["stg"])
    DMA(stg[O_N1:O_N1 + 8, :], n1w[:, :], [], ["stg"])
    DMA(stg[O_N2:O_N2 + 8, :], n2w[:, :], [], ["stg"])
    DMA(stg[O_BA:O_BA + 48, :], b_ada.rearrange("(r p) -> r p", p=128), [], ["stg"])
    DMA(stg[O_LB:O_LB + 8, :], lbl[:, :], [], ["stg"])
    TR(pbank(4)[:, 0:128], stg[:], ident_f[:], ["stg", "ident_f"], ["pb4"])
    CP("dve", stgT[:], pbank(4)[:, 0:NST], ["pb4"], ["stgT"])
    cT = stgT[:, O_C:O_C + 8 * NSEQ]
    ACT(siltmp[:], cT, AF.Exp, ["stgT"], ["siltmp"], scale=-1.0)
    TS("dve", siltmp[:], siltmp[:], 1.0, None, ALU.add, None, ["siltmp"], ["siltmp"])
    RECIP(siltmp[:], siltmp[:], ["siltmp"], ["siltmp"])
    TT("dve", silf[:], cT, siltmp[:], ALU.mult, ["stgT", "siltmp"], ["silf"])
    CP("dve", scT_b[:].rearrange("p k s -> p s k"), silf[:].rearrange("p (s k) -> p s k", k=8), ["silf"], ["scT_b"])

    def make_scl(which, jsc, onw):
        for cb in range(8):
            TS("dve", scl[:, which, cb, :], modT[:, jsc, cb, :], 1.0, stgT[:, onw + cb:onw + cb + 1], ALU.add, ALU.mult,
               ["modT", "stgT"], ["scl"])

    def norm_stats(src_tile, key, col):
        ACT(junk_big[:], src_tile, AF.Square, [key], ["gmx", "ssq%d" % col], accum_out=ssq[:, col:col + 1])

    def norm_rstd(c0, n, dim):
        TS("dve", rstd[:, c0:c0 + n], ssq[:, c0:c0 + n], 1.0 / dim, 1e-6, ALU.mult, ALU.add, ["ssq%d" % c for c in range(c0, c0 + n)], ["rstd%d" % c for c in range(c0, c0 + n)])
        ACT(rstd[:, c0:c0 + n], rstd[:, c0:c0 + n], AF.Ln, ["rstd%d" % c for c in range(c0, c0 + n)], ["rstd%d" % c for c in range(c0, c0 + n)])
        ACT(rstd[:, c0:c0 + n], rstd[:, c0:c0 + n], AF.Exp, ["rstd%d" % c for c in range(c0, c0 + n)], ["rstd%d" % c for c in range(c0, c0 + n)], scale=-0.5)

    def norm_apply_T(src_tile, key, col, s, which, jsh, uT_, ukey, col0):
        TS("dve", src_tile, src_tile, rstd[:, col:col + 1], None, ALU.mult, None, [key, "rstd%d" % col], [key])
        for g in range(2):
            for k in range(g * 4, g * 4 + 4):
                TR(pbank(4)[:, (k % 4) * 128:(k % 4 + 1) * 128], src_tile[:, k * 128:(k + 1) * 128], ident_f[:], [key, "ident_f"], ["pb4"])
            for k in range(g * 4, g * 4 + 4):
                ACT(uT_[:, k, col0:col0 + 128], pbank(4)[:, (k % 4) * 128:(k % 4 + 1) * 128], AF.Identity, ["pb4", "scl", "modT"], [ukey],
                    scale=scl[:, which, k, s:s + 1], bias=modT[:, jsh, k, s:s + 1])

    A.off = phase_base
    w_in_b = A.alloc("w_in_b", [128, 8, INW], BF16)
    w_out_b = A.alloc("w_out_b", [128, 8, D], BF16)
    kT = [A.alloc("kT%d" % h, [128, T], BF16) for h in range(4)]
    vaug = A.alloc("vaug", [128, NKB, 4, 130], BF16)
    gb = [A.alloc("gb%d" % s, [128, D], F32) for s in range(NSEQ)]
    lbo = A.alloc("lbo", [128, 8], F32)
    gnw_b = A.alloc("gnw_b", [128, 128], F32)
    sub_b = A.alloc("sub_b", [128, 128], F32)
    lamv = A.alloc("lamv", [128, 4], F32)
    neglam = A.alloc("neglam", [128, 1], F32)
    abias = A.alloc("abias", [128, 4, 16], F32)
    iot = A.alloc("iot", [128, 16], F32)
    mask2 = A.alloc("mask2", [128, 64], F32)
    xt = [A.alloc("xt%d" % i, [128, D], F32) for i in range(2)]
    hres = A.alloc("hres", [128, D], F32)
    gmx = A.alloc("gmx", [128, D], F32)
    junk_big = gmx
    uT2 = [A.alloc("uT%d" % i, [128, 8, TB], BF16) for i in range(2)]
    qTa = [A.alloc("qTa%d" % h, [128, TB], BF16) for h in range(4)]
    scanmask = A.alloc("scanmask", [128, TB], F32)
    qtil = [A.alloc("qtil%d" % i, [128, TB], BF16) for i in range(2)]
    ktilT = [A.alloc("ktilT%d" % i, [128, TB], BF16) for i in range(2)]
    ktil_tok = [A.alloc("ktil_tok%d" % i, [128, NSUB, 128], BF16) for i in range(2)]
    erbl = [A.alloc("erbl%d" % i, [128, NCH, 2], F32) for i in range(2)]
    elast = [A.alloc("elast%d" % i, [128, NCH], F32) for i in range(2)]
    S = A.alloc("S", [128, 4, 128], F32)
    Sr = A.alloc("Sr", [128, 2, 128], BF16)
    kvt2 = [A.alloc("kvt%d" % i, [128, 2, 128], F32) for i in range(2)]
    AT_sb2 = [A.alloc("AT_sb%d" % i, [128, 64], BF16) for i in range(2)]
    NPT = 3
    PT = [A.alloc("PT%d" % i, [128, 512], BF16) for i in range(NPT)]
    acc_sb = A.alloc("acc_sb", [128, 2 * NSUB, 130], F32)
    o_f = A.alloc("o_f", [128, NSUB, 128], F32)
    o_b = A.alloc("o_b", [128, NSUB, 128], BF16)
    rl = A.alloc("rl", [128, 2 * NSUB], F32)
    sgtmp = A.alloc("sgtmp", [128, 512], F32)
    o_sq = sgtmp[:, 0:NSUB * 128].rearrange("p (q v) -> p q v", v=128)
    tail0 = A.off
    t_sig = A.alloc("t_sig", [128, TB], F32)
    t_lf = A.alloc("t_lf", [128, TB], F32)
    t_omf = A.alloc("t_omf", [128, TB], F32)
    t_b = A.alloc("t_b", [128, TB], F32)
    t_E = A.alloc("t_E", [128, TB], F32)
    t_Ei = A.alloc("t_Ei", [128, TB], F32)
    v_tok = A.alloc("v_tok", [128, NSUB, 512], BF16)
    sg_tok = A.alloc("sg_tok", [128, NSUB, 512], BF16)
    mixT = A.alloc("mixT", [128, 8, TB], BF16)
    endA = A.off
    A.off = tail0
    wada_t = [A.alloc("wadaA%d" % i, [128, 8, 512], BF16) for i in range(2)]
    screp = [A.alloc("screp%d" % s, [128, 8, 128], BF16) for s in range(NSEQ)]
    bada_b = A.alloc("bada_b", [128, D], F32)
    lam4 = A.alloc("lam4", [128, 4, 64], F32)
    assert A.off <= max(endA, A.off) <= A.limit
    g2d = nc.dram_tensor("g2_scratch", [NSEQ, D], F32, kind="Internal").ap()

    for s in range(NSEQ):
        for k in range(8):
            TS("dve", screp[s][:, k, :], ones_f[:], silf[:, s * 8 + k:s * 8 + k + 1], None, ALU.mult, None,
               ["ones_f", "silf"], ["screp%d" % s])
    for j in (0, 1, 2, 3, 4, 5):
        for half in range(2):
            DMA(wada_t[half][:], w_ada[:, j * D + half * 512: j * D + half * 512 + 512].rearrange("(k p) n -> p k n", p=128),
                [], ["wada%d" % half], eng="pool")
        if j not in (2, 5):
            for cb in range(8):
                half, lc = cb // 4, (cb % 4) * 128
                col = (j * 8 + cb) * NSEQ
                for k in range(8):
                    MM(pbank(4)[:, col:col + NSEQ], wada_t[half][:, k, lc:lc + 128], scT_b[:, k, :], k == 0, k == 7,
                       ["wada%d" % half, "scT_b"], ["pb4"])
            for cb in range(8):
                col = (j * 8 + cb) * NSEQ
                TS("dve", modT[:, j, cb, :], pbank(4)[:, col:col + NSEQ], stgT[:, O_BA + j * 8 + cb:O_BA + j * 8 + cb + 1], None,
                   ALU.add, None, ["pb4", "stgT"], ["modT"])
        else:
            DMA(bada_b[:], b_ada[j * D:(j + 1) * D].partition_broadcast(128), [], ["bada_b"])
            for s in range(NSEQ):
                dst = gb[s] if j == 2 else gmx
                dkey = ("gb%d" % s) if j == 2 else "gmx"
                for half in range(2):
                    for k in range(8):
                        MM(pbank(half)[:], screp[s][:, k, :], wada_t[half][:, k, :], k == 0, k == 7,
                           ["screp%d" % s, "wada%d" % half], ["pb%d" % half])
                    TT("dve", dst[:, half * 512:(half + 1) * 512], pbank(half)[:], bada_b[:, half * 512:(half + 1) * 512], ALU.add,
                       ["pb%d" % half, "bada_b"], [dkey])
                if j == 5:
                    DMA(g2d[s:s + 1, :], gmx[0:1, :], ["gmx"], ["g2d"])
    make_scl(0, 1, O_N1)
    make_scl(1, 4, O_N2)
    for cc in range(INW // 512):
        DMA(w_in_b[:, :, cc * 512:(cc + 1) * 512], w_in[:, cc * 512:(cc + 1) * 512].rearrange("(k p) n -> p k n", p=128),
            [], ["w_in_b"], eng="pool")
    for cc in range(2):
        DMA(w_out_b[:, :, cc * 512:(cc + 1) * 512], w_out[:, cc * 512:(cc + 1) * 512].rearrange("(k p) n -> p k n", p=128),
            [], ["w_out_b"], eng="pool")
    l0 = stgT[:, O_LB:O_LB + 4]
    l1 = stgT[:, O_LB + 4:O_LB + 8]
    TT("dve", lbo[:, 4:8], l1, l0, ALU.subtract, ["stgT"], ["lbo"])
    ACT(lbo[:, 4:8], lbo[:, 4:8], AF.Exp, ["lbo"], ["lbo"])
    TS("dve", lbo[:, 4:8], lbo[:, 4:8], 1.0, None, ALU.add, None, ["lbo"], ["lbo"])
    RECIP(lbo[:, 0:4], lbo[:, 4:8], ["lbo"], ["lbo"])
    TS("dve", lbo[:, 4:8], lbo[:, 0:4], -1.0, 1.0, ALU.mult, ALU.add, ["lbo"], ["lbo"])
    DMA(gnw_b[:], gnw.partition_broadcast(128), [], ["gnw_b"])
    DMA(sub_b[:], subw.partition_broadcast(128), [], ["sub_b"])
    TS("dve", sub_b[:], sub_b[:], 0.8, None, ALU.mult, None, ["sub_b"], ["sub_b"])
    for i, v in enumerate((lq1, lk1, lq2, lk2)):
        DMA(lam4[:, i, :], v.partition_broadcast(128), [], ["lam4"])
    TT("dve", lam4[:, 0, :], lam4[:, 0, :], lam4[:, 1, :], ALU.mult, ["lam4"], ["lam4"])
    TT("dve", lam4[:, 2, :], lam4[:, 2, :], lam4[:, 3, :], ALU.mult, ["lam4"], ["lam4"])
    P.op("dve", lambda e: e.reduce_sum(out=lamv[:, 0:1], in_=lam4[:, 0, :], axis=AX.X), ["lam4"], ["lamv"])
    P.op("dve", lambda e: e.reduce_sum(out=lamv[:, 1:2], in_=lam4[:, 2, :], axis=AX.X), ["lam4"], ["lamv"])
    ACT(lamv[:, 0:2], lamv[:, 0:2], AF.Exp, ["lamv"], ["lamv"])
    TT("dve", neglam[:], lamv[:, 1:2], lamv[:, 0:1], ALU.subtract, ["lamv"], ["neglam"])
    TS("dve", neglam[:], neglam[:], -0.2, None, ALU.add, None, ["neglam"], ["neglam"])
    P.op("pool", lambda e: e.iota(iot[:], pattern=[[128, 16]], base=0, channel_multiplier=1, allow_small_or_imprecise_dtypes=True), [], ["iot"])
    W_H = [128, min(512, TB), min(512, TB), min(512, TB)]
    OFF_H = [15, 12, 12, 12]
    for h in range(4):
        slope = 2.0 ** (-2.0 * (h + 1))
        TS("dve", abias[:, h, :], iot[:], -128.0 * OFF_H[h], slope, ALU.add, ALU.mult, ["iot"], ["abias"])
    MEMSET("pool", vaug[:, :, :, 128:130], 1.0, ["vaug"])
    MEMSET("pool", scanmask[:], 1.0, ["scanmask"])
    MEMSET("pool", scanmask[:].rearrange("p (c t) -> p c t", t=64)[:, :, 0:1], 0.0, ["scanmask"])
    CP("dve", mask2[0:64, :], mask_f[0:64, 0:64], ["mask_f"], ["mask2"])
    CP("dve", mask2[64:128, :], mask_f[64:128, 64:128], ["mask_f"], ["mask2"])
    P.fence()
    MARK('setup_done')

    def post_norm_T(wb, wkey, extra, ekey, dst):
        TT("dve", o_sq, o_f[:], o_f[:], ALU.mult, ["o_f"], ["sgtmp"])
        P.op("dve", lambda e: e.reduce_sum(out=ssq[:, 4:4 + NSUB], in_=o_sq, axis=AX.X), ["sgtmp"], ["ssq%d" % c for c in range(4, 4 + NSUB)])
        norm_rstd(4, NSUB, 128)
        TT("dve", o_sq, o_f[:], rstd[:, 4:4 + NSUB].unsqueeze(2).to_broadcast([128, NSUB, 128]), ALU.mult,
           ["o_f"] + ["rstd%d" % c for c in range(4, 4 + NSUB)], ["sgtmp"])
        if extra is None:
            TT("dve", o_b[:], o_sq, wb[:].unsqueeze(1).to_broadcast([128, NSUB, 128]), ALU.mult, ["sgtmp", wkey], ["o_b"])
        else:
            TT("dve", o_sq, o_sq, wb[:].unsqueeze(1).to_broadcast([128, NSUB, 128]), ALU.mult, ["sgtmp", wkey], ["sgtmp"])
            TT("dve", o_b[:], o_sq, extra, ALU.mult, ["sgtmp", ekey], ["o_b"])
        for q in range(NSUB):
            TR(pbank(4)[:].bitcast(BF16)[:, q * 128:(q + 1) * 128], o_b[:, q, :], ident_b[:], ["o_b", "ident_b"], ["pb4"])
        CP("act", dst, pbank(4)[:].bitcast(BF16)[:, 0:NSUB * 128], ["pb4"], ["mixT"])

    HQ, HF, HI, HG, AQ, AK, AV = 0, 512, 1024, 1536, 2048, 2560, 3072
    pb_rr = [0]

    def next_pb():
        pb_rr[0] ^= 1
        return pb_rr[0]

    def proj_feat(uT_, ukey, col0, consume):
        b = next_pb()
        for k in range(8):
            MM(pbank(b)[:, 0:TB], w_in_b[:, k, col0:col0 + 128], uT_[:, k, :], k == 0, k == 7, ["w_in_b", ukey], ["pb%d" % b])
        consume(pbank(b)[:, 0:TB], "pb%d" % b)

    def proj_tok(uT_, ukey, col0, sub, consume):
        b = next_pb()
        for k in range(8):
            MM(pbank(b)[:], uT_[:, k, sub * 128:(sub + 1) * 128], w_in_b[:, k, col0:col0 + 512], k == 0, k == 7, ["w_in_b", ukey], ["pb%d" % b])
        consume(pbank(b)[:], "pb%d" % b)

    blocks = [(s, blk) for s in range(NSEQ) for blk in range(NBLK)]

    def emit_norm(bi):
        s, blk = blocks[bi]
        tok0 = s * T + blk * TB
        uT_ = uT2[bi % 2]
        ukey = "uT%d" % (bi % 2)
        for sub in range(NSUB):
            xb = xt[sub % 2]
            DMA(xb[:], x[tok0 + sub * 128: tok0 + (sub + 1) * 128, :], [], ["xt%d" % (sub % 2)])
            norm_stats(xb[:], "xt%d" % (sub % 2), sub % 2)
            norm_rstd(sub % 2, 1, D)
            norm_apply_T(xb[:], "xt%d" % (sub % 2), sub % 2, s, 0, 0, uT_, ukey, sub * 128)

    emit_norm(0)
    for bi, (s, blk) in enumerate(blocks):
        tok0 = s * T + blk * TB
        uT = uT2[bi % 2]
        ukey = "uT%d" % (bi % 2)
        if blk == 0:
            MEMSET("pool", S[:], 0.0, ["S"])
        for h in range(4):
            proj_feat(uT, ukey, AQ + h * 128, lambda ps, key, h=h: CP("act", qTa[h][:], ps, [key], ["qTa%d" % h]))
            proj_feat(uT, ukey, AK + h * 128, lambda ps, key, h=h: CP("dve", kT[h][:, blk * TB:(blk + 1) * TB], ps, [key], ["kT%d" % h]))
        for sub in range(NSUB):
            kb = blk * NSUB + sub
            proj_tok(uT, ukey, AV, sub, lambda ps, key, kb=kb: CP("act", vaug[:, kb, :, 0:128], ps.rearrange("p (h v) -> p h v", v=128), [key], ["vaug"]))
        MARK('attnproj_done')
        for sub in range(NSUB):
            proj_tok(uT, ukey, HI, sub, lambda ps, key, sub=sub: CP("act", v_tok[:, sub, :], ps, [key], ["v_tok"]))

            def gate(ps, key, sub=sub):
                ACT(sgtmp[:], ps, AF.Exp, [key], ["sgtmp"], scale=-1.0)
                ACT(sgtmp[:], sgtmp[:], AF.Ln, ["sgtmp"], ["sgtmp"], bias=1.0)
                ACT(sgtmp[:], sgtmp[:], AF.Exp, ["sgtmp"], ["sgtmp"], scale=-1.0)
                TT("dve", sg_tok[:, sub, :], ps, sgtmp[:], ALU.mult, [key, "sgtmp"], ["sg_tok"])
            proj_tok(uT, ukey, HG, sub, gate)
        MARK('vgate_done')
        for h in range(4):
            W = W_H[h]
            nqt = TB // W
            last_kb = blk * NSUB + NSUB - 1
            tiles = []
            for kb in range(last_kb + 1):
                for m in range(2):
                    for qt in range(nqt):
                        gq0 = blk * TB + qt * W
                        if kb * 128 >= gq0 + W:
                            continue
                        lo = max(gq0, kb * 128)
                        tiles.append((kb, m, lo - blk * TB, gq0 + W - lo, kb - gq0 // 128 + OFF_H[h], lo == kb * 128))
            acc_started = set()
            LA = int(_os2.environ.get('KLA', '2'))
            SB = [2, 3, 0, 1]
            for i in range(len(tiles) + LA):
                if i < len(tiles):
                    kb, m, lq, nq, d, diag = tiles[i]
                    sb_ = SB[i % 4]
                    pt = PT[i % NPT]
                    ptk = "PT%d" % (i % NPT)
                    MM(pbank(sb_)[:, 0:nq], kT[h][m * 64:(m + 1) * 64, kb * 128:(kb + 1) * 128],
                       qTa[h][m * 64:(m + 1) * 64, lq:lq + nq], True, True, ["kT%d" % h, "qTa%d" % h], ["pb%d" % sb_])
                    ACT(pt[:, 0:nq], pbank(sb_)[:, 0:nq], AF.Exp, ["pb%d" % sb_, "abias"], [ptk], scale=0.125, bias=abias[:, h, d:d + 1])
                    if diag:
                        TT("pool", pt[:, 0:128], pt[:, 0:128], mask_b[:], ALU.mult, [ptk, "mask_b"], [ptk])
                ip = i - LA
                if ip >= 0:
                    kb, m, lq, nq, d, diag = tiles[ip]
                    pt = PT[ip % NPT]
                    ptk = "PT%d" % (ip % NPT)
                    for j in range(nq // 128):
                        qb = (lq // 128) + j
                        a = m * NSUB + qb
                        ab, asl = 5 + a // 3, (a % 3) * 130
                        first = ab not in acc_started
                        acc_started.add(ab)
                        MM(pbank(ab)[:, asl:asl + 130], pt[:, j * 128:(j + 1) * 128], vaug[:, kb, h, :], first,
                           kb == blk * NSUB + qb, [ptk, "vaug"], ["pb%d" % ab], skip_group_check=True)
            for a in range(2 * NSUB):
                accp = pbank(5 + a // 3)[:, (a % 3) * 130:(a % 3) * 130 + 130]
                CP("act", acc_sb[:, a, :], accp, ["pb%d" % (5 + a // 3)], ["acc_sb"])
            RECIP(rl[:], acc_sb[:, :, 128], ["acc_sb"], ["rl"])
            TS("dve", rl[:, NSUB:2 * NSUB], rl[:, NSUB:2 * NSUB], neglam[:, 0:1], None, ALU.mult, None, ["rl", "neglam"], ["rl"])
            TT("dve", acc_sb[:, :, 0:128], acc_sb[:, :, 0:128], rl[:].unsqueeze(2).to_broadcast([128, 2 * NSUB, 128]), ALU.mult,
               ["acc_sb", "rl"], ["acc_sb"])
            TT("dve", o_f[:], acc_sb[:, 0:NSUB, 0:128], acc_sb[:, NSUB:2 * NSUB, 0:128], ALU.add, ["acc_sb"], ["o_f"])
            post_norm_T(sub_b, "sub_b", None, None, mixT[:, 4 + h, :])
        MARK('attn_done')
        if bi + 1 < len(blocks):
            emit_norm(bi + 1)
        def prep(h):
            i = h % 2

            def fchain(ps, key):
                ACT(t_sig[:], ps, AF.Exp, [key], ["t_sig"], scale=-1.0)
                ACT(t_sig[:], t_sig[:], AF.Ln, ["t_sig"], ["t_sig"], bias=1.0)
                ACT(t_sig[:], t_sig[:], AF.Exp, ["t_sig"], ["t_sig"], scale=-1.0)
                TS("dve", t_sig[:], t_sig[:], lbo[:, 4 + h:5 + h], lbo[:, h:h + 1], ALU.mult, ALU.add, ["t_sig", "lbo"], ["t_sig"])
                ACT(t_lf[:], t_sig[:], AF.Ln, ["t_sig"], ["t_lf"])
                TS("pool", t_omf[:], t_sig[:], -1.0, 1.0, ALU.mult, ALU.add, ["t_sig"], ["t_omf"])
                P.op("dve", lambda e: e.tensor_tensor_scan(out=t_b[:], data0=scanmask[:], data1=t_lf[:], initial=0.0,
                                                           op0=ALU.mult, op1=ALU.add), ["scanmask", "t_lf"], ["t_b"])
                b3 = t_b[:].rearrange("p (c t) -> p c t", t=64)
                ACT(erbl[i][:], b3[:, :, 31:64:32], AF.Exp, ["t_b"], ["erbl%d" % i])
                TT("dve", t_lf[:].rearrange("p (c t) -> p c t", t=64), b3, b3[:, :, 31:32].to_broadcast([128, NCH, 64]), ALU.subtract,
                   ["t_b"], ["t_lf"])
                ACT(t_E[:], t_lf[:], AF.Exp, ["t_lf"], ["t_E"])
                CP("pool", elast[i][:], t_E[:].rearrange("p (c t) -> p c t", t=64)[:, :, 63], ["t_E"], ["elast%d" % i])
                ACT(t_Ei[:], t_lf[:], AF.Exp, ["t_lf"], ["t_Ei"], scale=-1.0)
                TT("pool", ktilT[i][:], t_omf[:], t_Ei[:], ALU.mult, ["t_omf", "t_Ei"], ["ktilT%d" % i])
            proj_feat(uT, ukey, HF + h * 128, fchain)
            proj_feat(uT, ukey, HQ + h * 128, lambda ps, key: TT("dve", qtil[i][:], ps, t_E[:], ALU.mult, [key, "t_E"], ["qtil%d" % i]))
            for sub in range(NSUB):
                TR(pbank(4)[:].bitcast(BF16)[:, 0:128], ktilT[i][:, sub * 128:(sub + 1) * 128], ident_b[:], ["ktilT%d" % i, "ident_b"], ["pb4"])
                CP("act", ktil_tok[i][:, sub, :], pbank(4)[:].bitcast(BF16)[:, 0:128], ["pb4"], ["ktil_tok%d" % i])

        def rec(h):
            i = h % 2
            qk, kk, tk = "qtil%d" % i, "ktilT%d" % i, "ktil_tok%d" % i
            for sub in range(NSUB):
                ob = 3 if sub % 2 == 0 else 7
                okey = "pb%d" % ob
                kvt, AT_sb = kvt2[sub % 2], AT_sb2[sub % 2]
                kvk, atk = "kvt%d" % (sub % 2), "AT_sb%d" % (sub % 2)
                for half in range(2):
                    c = sub * 2 + half
                    r0 = half * 64
                    cs = slice(c * 64, (c + 1) * 64)
                    MM(pbank(5)[r0:r0 + 64, 0:64], ktilT[i][:, cs], qtil[i][:, cs], True, True, [kk, qk], ["pb5"], tile_position=(0, r0))
                for half in range(2):
                    r0 = half * 64
                    kvb = 6 if half == 0 else 2
                    MM(pbank(kvb)[:, 0:128], ktil_tok[i][r0:r0 + 64, sub, :], v_tok[r0:r0 + 64, sub, h * 128:(h + 1) * 128],
                       True, True, [tk, "v_tok"], ["pb%d" % kvb])
                TT("dve", AT_sb[:], pbank(5)[:, 0:64], mask2[:], ALU.mult, ["pb5", "mask2"], [atk])
                for half in range(2):
                    kvb = 6 if half == 0 else 2
                    TS("dve", kvt[:, half, :], pbank(kvb)[:, 0:128], elast[i][:, 2 * sub + half:2 * sub + half + 1], None, ALU.mult, None,
                       ["pb%d" % kvb, "elast%d" % i], [kvk])
                for half in range(2):
                    c = sub * 2 + half
                    r0 = half * 64
                    cs = slice(c * 64, (c + 1) * 64)
                    TS("dve", Sr[:, half, :], S[:, h, :], erbl[i][:, c, 0:1], None, ALU.mult, None, ["S", "erbl%d" % i], ["Sr%d" % half])
                    MM(pbank(ob)[r0:r0 + 64, 0:128], qtil[i][:, cs], Sr[:, half, :], True, False, [qk, "Sr%d" % half], [okey], tile_position=(0, r0))
                    MM(pbank(ob)[r0:r0 + 64, 0:128], AT_sb[r0:r0 + 64, :], v_tok[r0:r0 + 64, sub, h * 128:(h + 1) * 128], False, True,
                       [atk, "v_tok"], [okey], tile_position=(r0, r0))
                    STT("dve", S[:, h, :], S[:, h, :], erbl[i][:, c, 1:2], kvt[:, half, :], ALU.mult, ALU.add, ["S", "erbl%d" % i, kvk], ["S"])
                CP("act", o_f[:, sub, :], pbank(ob)[:, 0:128], [okey], ["o_f"])
            post_norm_T(gnw_b, "gnw_b", sg_tok[:, :, h * 128:(h + 1) * 128], "sg_tok", mixT[:, h, :])

        MARK('norm_next_done')
        prep(0)
        prep(1)
        MARK('prep01_done')
        rec(0)
        MARK('rec0_done')
        prep(2)
        rec(1)
        prep(3)
        rec(2)
        rec(3)
        MARK('hgrn_done')
        for sub in range(NSUB):
            DMA(hres[:], x[tok0 + sub * 128: tok0 + (sub + 1) * 128, :], [], ["hres"])
            for half in range(2):
                b = 5 + half
                for k in range(8):
                    MM(pbank(b)[:], mixT[:, k, sub * 128:(sub + 1) * 128], w_out_b[:, k, half * 512:(half + 1) * 512], k == 0, k == 7,
                       ["mixT", "w_out_b"], ["pb%d" % b])
                TT("dve", gmx[:, half * 512:(half + 1) * 512], pbank(b)[:], gb[s][:, half * 512:(half + 1) * 512], ALU.mult,
                   ["pb%d" % b, "gb%d" % s], ["gmx"])
            TT("pool", hres[:], hres[:], gmx[:], ALU.add, ["hres", "gmx"], ["hres"])
            DMA(h1[tok0 + sub * 128: tok0 + (sub + 1) * 128, :], hres[:], ["hres"], ["h1dram"])
    P.fence()

    MARK('phaseA_done')
    NSB = TBB // 128
    A.off = phase_base
    w_g_b = A.alloc("w_g_b", [128, 8, DFF], BF16)
    w_u_b = A.alloc("w_u_b", [128, 8, DFF], BF16)
    w_d_b = A.alloc("w_d_b", [128, NKF, D], BF16)
    fnw_b = A.alloc("fnw_b", [128, D], F32)
    g2b = A.alloc("g2b", [128, D], F32)
    ht = [A.alloc("ht%d" % i, [128, D], F32) for i in range(NSB)]
    u2T = A.alloc("u2T", [128, 8, TBB], BF16)
    hidT = A.alloc("hidT", [128, NKF, TBB], BF16)
    sgb_all = A.alloc("sgb_all", [128, 2, 512], F32)
    sgb = [sgb_all[:, i, 0:TBB] for i in range(2)]
    sq_junk = sgb_all[:].rearrange("p a b -> p (a b)")
    h2 = [A.alloc("h2_%d" % i, [128, D], F32) for i in range(2)]
    ffg = A.alloc("ffg", [128, D], F32)

    for cc in range(0, DFF, 512):
        w = min(512, DFF - cc)
        DMA(w_g_b[:, :, cc:cc + w], w_g[:, cc:cc + w].rearrange("(k p) n -> p k n", p=128), [], ["w_g_b%d" % (cc // 512)], eng="pool")
        DMA(w_u_b[:, :, cc:cc + w], w_u[:, cc:cc + w].rearrange("(k p) n -> p k n", p=128), [], ["w_u_b%d" % (cc // 512)], eng="pool")
    for k2 in range(0, NKF, 2):
        DMA(w_d_b[:, k2:k2 + 2, :], w_d[k2 * 128:(k2 + 2) * 128, :].rearrange("(k p) n -> p k n", p=128), [], ["w_d_b%d" % (k2 // 2)], eng="pool")
    DMA(fnw_b[:], fnw.partition_broadcast(128), [], ["fnw_b"])

    bblocks = [(s, blk) for s in range(NSEQ) for blk in range(T // TBB)]

    def b_norm(bi):
        s, blk = bblocks[bi]
        tok0 = s * T + blk * TBB
        for sub in range(NSB):
            DMA(ht[sub][:], h1[tok0 + sub * 128: tok0 + (sub + 1) * 128, :], ["h1dram"], ["ht%d" % sub])
            ACT(sq_junk, ht[sub][:], AF.Square, ["ht%d" % sub], ["sgb0", "sgb1", "ssq%d" % sub], accum_out=ssq[:, sub:sub + 1])
        norm_rstd(0, NSB, D)
        for sub in range(NSB):
            norm_apply_T(ht[sub][:], "ht%d" % sub, sub, s, 1, 3, u2T, "u2T", sub * 128)

    def b_down(bi, sub):
        s, blk = bblocks[bi]
        tok0 = s * T + blk * TBB
        hb = h2[sub % 2]
        hk = "h2_%d" % (sub % 2)
        DMA(hb[:], h1[tok0 + sub * 128: tok0 + (sub + 1) * 128, :], ["h1dram"], [hk])
        for half in range(2):
            b = 4 + (2 * sub + half) % 4
            for f in range(NKF):
                MM(pbank(b)[:], hidT[:, f, sub * 128:(sub + 1) * 128], w_d_b[:, f, half * 512:(half + 1) * 512], f == 0, f == NKF - 1,
                   ["hidT", "w_d_b%d" % (f // 2)], ["pb%d" % b])
        for half in range(2):
            b = 4 + (2 * sub + half) % 4
            TT("dve", ffg[:, half * 512:(half + 1) * 512], pbank(b)[:], g2b[:, half * 512:(half + 1) * 512], ALU.mult,
               ["pb%d" % b, "g2b"], ["ffg"])
        TT("pool", hb[:], hb[:], ffg[:], ALU.add, [hk, "ffg"], [hk])
        ACT(ffg[:], hb[:], AF.Square, [hk], ["ffg", "ssq6"], accum_out=ssq[:, 6:7])
        norm_rstd(6, 1, D)
        STT("dve", hb[:], hb[:], rstd[:, 6:7], fnw_b[:], ALU.mult, ALU.mult, [hk, "rstd6", "fnw_b"], [hk])
        DMA(out[tok0 + sub * 128: tok0 + (sub + 1) * 128, :], hb[:], [hk], ["outdram"])

    MARK('phaseB_loads')
    b_norm(0)
    MARK('bnorm0')
    for bi, (s, blk) in enumerate(bblocks):
        if blk == 0:
            DMA(g2b[:], g2d[s, :].partition_broadcast(128), ["g2d"], ["g2b"])
        for f in range(NKF):
            bg, bu = (0, 1) if f % 2 == 0 else (2, 3)
            for k in range(8):
                MM(pbank(bg)[:, 0:TBB], w_g_b[:, k, f * 128:(f + 1) * 128], u2T[:, k, :], k == 0, k == 7, ["w_g_b%d" % (f // 4), "u2T"], ["pb%d" % bg])
            for k in range(8):
                MM(pbank(bu)[:, 0:TBB], w_u_b[:, k, f * 128:(f + 1) * 128], u2T[:, k, :], k == 0, k == 7, ["w_u_b%d" % (f // 4), "u2T"], ["pb%d" % bu])
            ACT(sgb[f % 2], pbank(bg)[:, 0:TBB], AF.Silu, ["pb%d" % bg], ["sgb%d" % (f % 2)])
            TT("dve", hidT[:, f, :], pbank(bu)[:, 0:TBB], sgb[f % 2], ALU.mult, ["pb%d" % bu, "sgb%d" % (f % 2)], ["hidT"])
        MARK('gu_done')
        for sub in range(NSB):
            b_down(bi, sub)
            MARK('down%d' % sub)
            if sub == (NSB - 1) // 2 and bi + 1 < len(bblocks):
                b_norm(bi + 1)

    import os as _os
    _n = int(_os.environ.get('KSTOP', '0'))
    if _n:
        P.ops = P.ops[:_n]
    print('NOPS', len(P.ops))
    P.analyze()
    esems = {e: nc.alloc_semaphore("es_" + e) for e in ENGS}
    dsems = [nc.alloc_semaphore("ds%d" % i) for i in range(P.n_dma_sems)]
    with nc.Block() as block:
        P.emit(block, esems, dsems)
    return nc


def core_inputs(inputs, core, NSEQ, T):
    f = lambda a: np.ascontiguousarray(np.asarray(a, dtype=np.float32))
    sl = slice(core * NSEQ, (core + 1) * NSEQ)
    return {
        "x": f(np.asarray(inputs["x"])[sl].reshape(NSEQ * T, D)),
        "c": f(np.asarray(inputs["c"])[sl].reshape(NSEQ * 8, 128)),
        "w_ada": f(np.asarray(inputs["w_ada"])[0]),
        "b_ada": f(np.asarray(inputs["b_ada"])[0]),
        "norm1_w": f(np.asarray(inputs["norm1_w"])[0].reshape(8, 128)),
        "w_in": f(np.asarray(inputs["w_in"])[0]),
        "lb_logits": f(np.asarray(inputs["hgrn_lb_logits"]).reshape(8, 128)),
        "gnorm_w": f(np.asarray(inputs["hgrn_gnorm_w"])[0]),
        "lq1": f(np.asarray(inputs["diff_lambda_q1"])[0]),
        "lk1": f(np.asarray(inputs["diff_lambda_k1"])[0]),
        "lq2": f(np.asarray(inputs["diff_lambda_q2"])[0]),
        "lk2": f(np.asarray(inputs["diff_lambda_k2"])[0]),
        "subln_w": f(np.asarray(inputs["diff_subln_w"])[0]),
        "w_out": f(np.asarray(inputs["w_out"])[0]),
        "norm2_w": f(np.asarray(inputs["norm2_w"])[0].reshape(8, 128)),
        "w_g": f(np.asarray(inputs["w_ffn_gate"])[0]),
        "w_u": f(np.asarray(inputs["w_ffn_up"])[0]),
        "w_d": f(np.asarray(inputs["w_ffn_down"])[0]),
        "fnw": f(np.asarray(inputs["final_norm_w"])),
    }


def kernel(**inputs):
    B, T, _ = np.asarray(inputs["x"]).shape
    n = 8
    NSEQ = B // n
    nc = build(T=T, NSEQ=NSEQ)
    in_maps = [core_inputs(inputs, i, NSEQ, T) for i in range(n)]
    res = run_bass_kernel_spmd(nc, in_maps, core_ids=list(range(n)))
    outs = [np.asarray(r["out"]).reshape(NSEQ, T, D) for r in res.results]
    return np.concatenate(outs, axis=0).astype(np.float32)
```

```python
import numpy as np
import os as _os2
import concourse.bass as bass
import concourse.mybir as mybir
from concourse.bass_utils import run_bass_kernel_spmd

F32 = mybir.dt.float32
BF16 = mybir.dt.bfloat16
AF = mybir.ActivationFunctionType
ALU = mybir.AluOpType
AX = mybir.AxisListType

D = 1024
DFF = 2816
NKF = DFF // 128
INW = 3584
ENGS = ("pe", "act", "dve", "pool", "sp")


class Op:
    __slots__ = ("eng", "fn", "reads", "writes", "dma", "waits", "mark", "seq", "dsem", "dval", "idx", "fence")

    def __init__(self, eng, fn, reads, writes, dma):
        self.eng = eng
        self.fn = fn
        self.reads = tuple(reads)
        self.writes = tuple(writes)
        self.dma = dma
        self.waits = []
        self.mark = False
        self.seq = 0
        self.dsem = None
        self.dval = 0
        self.fence = False


class Prog:
    def __init__(self, n_dma_sems=24):
        self.ops = []
        self.n_dma_sems = n_dma_sems

    def op(self, eng, fn, reads=(), writes=(), dma=False):
        o = Op(eng, fn, reads, writes, dma)
        o.idx = len(self.ops)
        self.ops.append(o)
        return o

    def fence(self):
        o = Op("sp", None, (), (), False)
        o.idx = len(self.ops)
        o.fence = True
        self.ops.append(o)

    def analyze(self):
        last_writer = {}
        readers = {}
        need = []
        last_eng = {e: None for e in ENGS}
        pend_dma = []
        fence_deps = []
        for o in self.ops:
            if o.fence:
                fence_deps = [j for j in last_eng.values() if j is not None] + list(pend_dma)
                pend_dma = []
                last_writer = {}
                readers = {}
                need.append([])
                continue
            deps = {}
            for j in fence_deps:
                deps[j] = "fence"
            for r in o.reads:
                j = last_writer.get(r)
                if j is not None:
                    deps[j] = "raw"
            for w in o.writes:
                j = last_writer.get(w)
                if j is not None and j not in deps:
                    deps[j] = "waw"
                for j in readers.get(w, ()):
                    if j not in deps and j != o.idx:
                        deps[j] = "war"
            for r in o.reads:
                readers.setdefault(r, []).append(o.idx)
            for w in o.writes:
                last_writer[w] = o.idx
                readers[w] = []
            nd = []
            for j, kind in deps.items():
                p = self.ops[j]
                if p.dma:
                    nd.append(j)
                elif p.eng == o.eng:
                    if o.dma:
                        p.mark = True
                        nd.append(j)
                    elif p.eng == "pe":
                        continue
                    elif kind != "fence":
                        p.mark = True
                        nd.append(j)
                else:
                    p.mark = True
                    nd.append(j)
            need.append(nd)
            if o.dma:
                pend_dma.append(o.idx)
            else:
                last_eng[o.eng] = o.idx
        for e, j in last_eng.items():
            if j is not None:
                self.ops[j].mark = True
        cnt = {e: 0 for e in ENGS}
        dcnt = [0] * self.n_dma_sems
        k = 0
        kp = 0
        for o in self.ops:
            if o.fence:
                continue
            if o.dma:
                if o.eng == "pool":
                    o.dsem = kp % 8
                    kp += 1
                else:
                    o.dsem = 8 + k % (self.n_dma_sems - 8)
                    k += 1
                dcnt[o.dsem] += 1
                o.dval = 16 * dcnt[o.dsem]
            elif o.mark:
                cnt[o.eng] += 1
                o.seq = cnt[o.eng]
        waited = {e: {} for e in ENGS}
        for o in self.ops:
            if o.fence:
                continue
            w = {}
            for j in need[o.idx]:
                p = self.ops[j]
                if p.dma:
                    key, val = ("d", p.dsem), p.dval
                else:
                    key, val = ("e", p.eng), p.seq
                if val > w.get(key, 0):
                    w[key] = val
            if o.dma and o.dval > 16:
                key = ("d", o.dsem)
                w[key] = max(w.get(key, 0), o.dval - 16)
            ws = waited[o.eng]
            for key, val in w.items():
                if ws.get(key, 0) < val:
                    ws[key] = val
                    o.waits.append((key, val))
        self.final_dma = [(i, 16 * c) for i, c in enumerate(dcnt) if c > 0]
        self.final_cnt = cnt

    def emit(self, block, esems, dsems):
        def run_engine(ename, eng):
            for o in self.ops:
                if o.fence or o.eng != ename:
                    continue
                for key, val in o.waits:
                    sem = dsems[key[1]] if key[0] == "d" else esems[key[1]]
                    eng.wait_ge(sem, val)
                ins = o.fn(eng)
                if o.dma:
                    ins.then_inc(dsems[o.dsem], 16)
                elif o.mark:
                    ins.then_inc(esems[o.eng], 1)
            if ename == "sp":
                for i, v in self.final_dma:
                    eng.wait_ge(dsems[i], v)
                for e in ENGS:
                    if e != "sp" and self.final_cnt[e] > 0:
                        eng.wait_ge(esems[e], self.final_cnt[e])

        block.tensor(lambda e: run_engine("pe", e))
        block.scalar(lambda e: run_engine("act", e))
        block.vector(lambda e: run_engine("dve", e))
        block.gpsimd(lambda e: run_engine("pool", e))
        block.sync(lambda e: run_engine("sp", e))


class Arena:
    def __init__(self, nc, base=16640, limit=229376):
        self.nc = nc
        self.off = base
        self.limit = limit
        self.n = 0

    def alloc(self, name, shape, dt):
        isz = 2 if dt == BF16 else 4
        size = isz
        for s in shape[1:]:
            size *= s
        size = (size + 63) // 64 * 64
        off = self.off
        assert off + size <= self.limit, ("SBUF overflow", name, off, size)
        self.off += size
        self.n += 1
        return self.nc.alloc_sbuf_tensor_at(name, list(shape), dt, offset=off)


def build(T=2048, NSEQ=2, TB=512, TBB=512):
    NSUB = TB // 128
    NBLK = T // TB
    NCH = TB // 64
    NKB = T // 128
    NTOK = NSEQ * T
    nc = bass.Bass("TRN2", target_bir_lowering=False)

    def din(name, shape):
        return nc.dram_tensor(name, list(shape), F32, kind="ExternalInput").ap()

    x = din("x", [NTOK, D])
    c_in = din("c", [NSEQ * 8, 128])
    w_ada = din("w_ada", [D, 6 * D])
    b_ada = din("b_ada", [6 * D])
    n1w = din("norm1_w", [8, 128])
    w_in = din("w_in", [D, INW])
    lbl = din("lb_logits", [8, 128])
    gnw = din("gnorm_w", [128])
    lq1 = din("lq1", [64])
    lk1 = din("lk1", [64])
    lq2 = din("lq2", [64])
    lk2 = din("lk2", [64])
    subw = din("subln_w", [128])
    w_out = din("w_out", [D, D])
    n2w = din("norm2_w", [8, 128])
    w_g = din("w_g", [D, DFF])
    w_u = din("w_u", [D, DFF])
    w_d = din("w_d", [DFF, D])
    fnw = din("fnw", [D])
    out = nc.dram_tensor("out", [NTOK, D], F32, kind="ExternalOutput").ap()
    h1 = nc.dram_tensor("h1_scratch", [NTOK, D], F32, kind="Internal").ap()

    P = Prog()

    def MARK(name):
        print('MARK', name, len(P.ops))
    A = Arena(nc)

    pbs = [nc.alloc_psum_tensor("pb%d" % i, [128, 512], F32) for i in range(8)]

    def pbank(i):
        return pbs[i][:]

    def MM(out_, lhsT, rhs, start, stop, reads, writes, **kw):
        P.op("pe", lambda e: e.matmul(out_, lhsT=lhsT, rhs=rhs, start=start, stop=stop, **kw), reads, writes)

    def TR(out_, in_, ident, reads, writes):
        P.op("pe", lambda e: e.transpose(out=out_, in_=in_, identity=ident), reads, writes)

    def ACT(out_, in_, func, reads, writes, bias=0.0, scale=1.0, accum_out=None):
        if accum_out is None:
            P.op("act", lambda e: e.activation(out=out_, in_=in_, func=func, bias=bias, scale=scale), reads, writes)
        else:
            P.op("act", lambda e: e.activation(out=out_, in_=in_, func=func, bias=bias, scale=scale, accum_out=accum_out), reads, writes)

    def TS(eng, out_, in0, s1, s2, op0, op1, reads, writes):
        if s2 is None:
            P.op(eng, lambda e: e.tensor_scalar(out=out_, in0=in0, scalar1=s1, scalar2=None, op0=op0), reads, writes)
        else:
            P.op(eng, lambda e: e.tensor_scalar(out=out_, in0=in0, scalar1=s1, scalar2=s2, op0=op0, op1=op1), reads, writes)

    def TT(eng, out_, in0, in1, op, reads, writes):
        P.op(eng, lambda e: e.tensor_tensor(out=out_, in0=in0, in1=in1, op=op), reads, writes)

    def STT(eng, out_, in0, scalar, in1, op0, op1, reads, writes):
        P.op(eng, lambda e: e.scalar_tensor_tensor(out=out_, in0=in0, scalar=scalar, in1=in1, op0=op0, op1=op1), reads, writes)

    def CP(eng, out_, in_, reads, writes):
        if eng == "act":
            P.op("act", lambda e: e.copy(out=out_, in_=in_), reads, writes)
        else:
            P.op(eng, lambda e: e.tensor_copy(out=out_, in_=in_), reads, writes)

    def RECIP(out_, in_, reads, writes):
        P.op("dve", lambda e: e.reciprocal(out=out_, in_=in_), reads, writes)

    def MEMSET(eng, ap, val, writes):
        P.op(eng, lambda e: e.memset(ap, val), (), writes)

    def DMA(out_, in_, reads, writes, eng="sp", **kw):
        P.op(eng, lambda e: e.dma_start(out=out_, in_=in_, **kw), reads, writes, dma=True)

    def rstd_from_ssq(ssq, rstd, n, key):
        TS("dve", rstd, ssq, 1.0 / n, 1e-6, ALU.mult, ALU.add, [key + "ssq"], [key + "rstd"])
        ACT(rstd, rstd, AF.Ln, [key + "rstd"], [key + "rstd"])
        ACT(rstd, rstd, AF.Exp, [key + "rstd"], [key + "rstd"], scale=-0.5)

    ident_f = A.alloc("ident_f", [128, 128], F32)
    ident_b = A.alloc("ident_b", [128, 128], BF16)
    ones_f = A.alloc("ones_f", [128, 128], F32)
    mask_f = A.alloc("mask_f", [128, 128], F32)
    mask_b = A.alloc("mask_b", [128, 128], BF16)
    NST = 8 * NSEQ + 8 + 8 + 48 + 8
    O_C, O_N1, O_N2, O_BA, O_LB = 0, 8 * NSEQ, 8 * NSEQ + 8, 8 * NSEQ + 16, 8 * NSEQ + 64
    stg = A.alloc("stg", [128, 128], F32)
    stgT = A.alloc("stgT", [128, NST], F32)
    modT = A.alloc("modT", [128, 6, 8, NSEQ], F32)
    scl = A.alloc("scl", [128, 2, 8, NSEQ], F32)
    silf = A.alloc("silf", [128, NSEQ * 8], F32)
    siltmp = A.alloc("siltmp", [128, NSEQ * 8], F32)
    scT_b = A.alloc("scT_b", [128, 8, NSEQ], BF16)
    ssq = A.alloc("ssq", [128, 8], F32)
    rstd = A.alloc("rstd", [128, 8], F32)
    phase_base = A.off

    MEMSET("pool", ident_f[:], 0.0, ["ident_f"])
    P.op("pool", lambda e: e.affine_select(out=ident_f[:], in_=ident_f[:], pattern=[[-1, 128]], compare_op=ALU.not_equal,
                                           fill=1.0, base=0, channel_multiplier=1), ["ident_f"], ["ident_f"])
    CP("pool", ident_b[:], ident_f[:], ["ident_f"], ["ident_b"])
    MEMSET("pool", ones_f[:], 1.0, ["ones_f"])
    P.op("pool", lambda e: e.affine_select(out=mask_f[:], in_=ones_f[:], pattern=[[1, 128]], compare_op=ALU.is_ge,
                                           fill=0.0, base=0, channel_multiplier=-1), ["ones_f"], ["mask_f"])
    CP("pool", mask_b[:], mask_f[:], ["mask_f"], ["mask_b"])
    MEMSET("dve", stg[:], 0.0, ["stg"])
    DMA(stg[O_C:O_C + 8 * NSEQ, :], c_in[:, :], [], ["stg"])
    DMA(stg[O_N1:O_N1 + 8, :], n1w[:, :], [], ["stg"])
    DMA(stg[O_N2:O_N2 + 8, :], n2w[:, :], [], ["stg"])
    DMA(stg[O_BA:O_BA + 48, :], b_ada.rearrange("(r p) -> r p", p=128), [], ["stg"])
    DMA(stg[O_LB:O_LB + 8, :], lbl[:, :], [], ["stg"])
    TR(pbank(4)[:, 0:128], stg[:], ident_f[:], ["stg", "ident_f"], ["pb4"])
    CP("dve", stgT[:], pbank(4)[:, 0:NST], ["pb4"], ["stgT"])
    cT = stgT[:, O_C:O_C + 8 * NSEQ]
    ACT(siltmp[:], cT, AF.Exp, ["stgT"], ["siltmp"], scale=-1.0)
    TS("dve", siltmp[:], siltmp[:], 1.0, None, ALU.add, None, ["siltmp"], ["siltmp"])
    RECIP(siltmp[:], siltmp[:], ["siltmp"], ["siltmp"])
    TT("dve", silf[:], cT, siltmp[:], ALU.mult, ["stgT", "siltmp"], ["silf"])
    CP("dve", scT_b[:].rearrange("p k s -> p s k"), silf[:].rearrange("p (s k) -> p s k", k=8), ["silf"], ["scT_b"])

    def make_scl(which, jsc, onw):
        for cb in range(8):
            TS("dve", scl[:, which, cb, :], modT[:, jsc, cb, :], 1.0, stgT[:, onw + cb:onw + cb + 1], ALU.add, ALU.mult,
               ["modT", "stgT"], ["scl"])

    def norm_stats(src_tile, key, col):
        ACT(junk_big[:], src_tile, AF.Square, [key], ["gmx", "ssq%d" % col], accum_out=ssq[:, col:col + 1])

    def norm_rstd(c0, n, dim):
        TS("dve", rstd[:, c0:c0 + n], ssq[:, c0:c0 + n], 1.0 / dim, 1e-6, ALU.mult, ALU.add, ["ssq%d" % c for c in range(c0, c0 + n)], ["rstd%d" % c for c in range(c0, c0 + n)])
        ACT(rstd[:, c0:c0 + n], rstd[:, c0:c0 + n], AF.Ln, ["rstd%d" % c for c in range(c0, c0 + n)], ["rstd%d" % c for c in range(c0, c0 + n)])
        ACT(rstd[:, c0:c0 + n], rstd[:, c0:c0 + n], AF.Exp, ["rstd%d" % c for c in range(c0, c0 + n)], ["rstd%d" % c for c in range(c0, c0 + n)], scale=-0.5)

    def norm_apply_T(src_tile, key, col, s, which, jsh, uT_, ukey, col0):
        TS("dve", src_tile, src_tile, rstd[:, col:col + 1], None, ALU.mult, None, [key, "rstd%d" % col], [key])
        for g in range(2):
            for k in range(g * 4, g * 4 + 4):
                TR(pbank(4)[:, (k % 4) * 128:(k % 4 + 1) * 128], src_tile[:, k * 128:(k + 1) * 128], ident_f[:], [key, "ident_f"], ["pb4"])
            for k in range(g * 4, g * 4 + 4):
                ACT(uT_[:, k, col0:col0 + 128], pbank(4)[:, (k % 4) * 128:(k % 4 + 1) * 128], AF.Identity, ["pb4", "scl", "modT"], [ukey],
                    scale=scl[:, which, k, s:s + 1], bias=modT[:, jsh, k, s:s + 1])

    A.off = phase_base
    w_in_b = A.alloc("w_in_b", [128, 8, INW], BF16)
    w_out_b = A.alloc("w_out_b", [128, 8, D], BF16)
    kT = [A.alloc("kT%d" % h, [128, T], BF16) for h in range(4)]
    vaug = A.alloc("vaug", [128, NKB, 4, 130], BF16)
    gb = [A.alloc("gb%d" % s, [128, D], F32) for s in range(NSEQ)]
    lbo = A.alloc("lbo", [128, 8], F32)
    gnw_b = A.alloc("gnw_b", [128, 128], F32)
    sub_b = A.alloc("sub_b", [128, 128], F32)
    lamv = A.alloc("lamv", [128, 4], F32)
    neglam = A.alloc("neglam", [128, 1], F32)
    abias = A.alloc("abias", [128, 4, 16], F32)
    iot = A.alloc("iot", [128, 16], F32)
    mask2 = A.alloc("mask2", [128, 64], F32)
    xt = [A.alloc("xt%d" % i, [128, D], F32) for i in range(2)]
    hres = A.alloc("hres", [128, D], F32)
    gmx = A.alloc("gmx", [128, D], F32)
    junk_big = gmx
    uT2 = [A.alloc("uT%d" % i, [128, 8, TB], BF16) for i in range(2)]
    qTa = [A.alloc("qTa%d" % h, [128, TB], BF16) for h in range(4)]
    scanmask = A.alloc("scanmask", [128, TB], F32)
    qtil = [A.alloc("qtil%d" % i, [128, TB], BF16) for i in range(2)]
    ktilT = [A.alloc("ktilT%d" % i, [128, TB], BF16) for i in range(2)]
    ktil_tok = [A.alloc("ktil_tok%d" % i, [128, NSUB, 128], BF16) for i in range(2)]
    erbl = [A.alloc("erbl%d" % i, [128, NCH, 2], F32) for i in range(2)]
    elast = [A.alloc("elast%d" % i, [128, NCH], F32) for i in range(2)]
    S = A.alloc("S", [128, 4, 128], F32)
    Sr = A.alloc("Sr", [128, 2, 128], BF16)
    kvt2 = [A.alloc("kvt%d" % i, [128, 2, 128], F32) for i in range(2)]
    AT_sb2 = [A.alloc("AT_sb%d" % i, [128, 64], BF16) for i in range(2)]
    NPT = 3
    PT = [A.alloc("PT%d" % i, [128, 512], BF16) for i in range(NPT)]
    acc_sb = A.alloc("acc_sb", [128, 2 * NSUB, 130], F32)
    o_f = A.alloc("o_f", [128, NSUB, 128], F32)
    o_b = A.alloc("o_b", [128, NSUB, 128], BF16)
    rl = A.alloc("rl", [128, 2 * NSUB], F32)
    sgtmp = A.alloc("sgtmp", [128, 512], F32)
    o_sq = sgtmp[:, 0:NSUB * 128].rearrange("p (q v) -> p q v", v=128)
    tail0 = A.off
    t_sig = A.alloc("t_sig", [128, TB], F32)
    t_lf = A.alloc("t_lf", [128, TB], F32)
    t_omf = A.alloc("t_omf", [128, TB], F32)
    t_b = A.alloc("t_b", [128, TB], F32)
    t_E = A.alloc("t_E", [128, TB], F32)
    t_Ei = A.alloc("t_Ei", [128, TB], F32)
    v_tok = A.alloc("v_tok", [128, NSUB, 512], BF16)
    sg_tok = A.alloc("sg_tok", [128, NSUB, 512], BF16)
    mixT = A.alloc("mixT", [128, 8, TB], BF16)
    endA = A.off
    A.off = tail0
    wada_t = [A.alloc("wadaA%d" % i, [128, 8, 512], BF16) for i in range(2)]
    screp = [A.alloc("screp%d" % s, [128, 8, 128], BF16) for s in range(NSEQ)]
    bada_b = A.alloc("bada_b", [128, D], F32)
    lam4 = A.alloc("lam4", [128, 4, 64], F32)
    assert A.off <= max(endA, A.off) <= A.limit
    g2d = nc.dram_tensor("g2_scratch", [NSEQ, D], F32, kind="Internal").ap()

    for s in range(NSEQ):
        for k in range(8):
            TS("dve", screp[s][:, k, :], ones_f[:], silf[:, s * 8 + k:s * 8 + k + 1], None, ALU.mult, None,
               ["ones_f", "silf"], ["screp%d" % s])
    for j in (0, 1, 2, 3, 4, 5):
        for half in range(2):
            DMA(wada_t[half][:], w_ada[:, j * D + half * 512: j * D + half * 512 + 512].rearrange("(k p) n -> p k n", p=128),
                [], ["wada%d" % half], eng="pool")
        if j not in (2, 5):
            for cb in range(8):
                half, lc = cb // 4, (cb % 4) * 128
                col = (j * 8 + cb) * NSEQ
                for k in range(8):
                    MM(pbank(4)[:, col:col + NSEQ], wada_t[half][:, k, lc:lc + 128], scT_b[:, k, :], k == 0, k == 7,
                       ["wada%d" % half, "scT_b"], ["pb4"])
            for cb in range(8):
                col = (j * 8 + cb) * NSEQ
                TS("dve", modT[:, j, cb, :], pbank(4)[:, col:col + NSEQ], stgT[:, O_BA + j * 8 + cb:O_BA + j * 8 + cb + 1], None,
                   ALU.add, None, ["pb4", "stgT"], ["modT"])
        else:
            DMA(bada_b[:], b_ada[j * D:(j + 1) * D].partition_broadcast(128), [], ["bada_b"])
            for s in range(NSEQ):
                dst = gb[s] if j == 2 else gmx
                dkey = ("gb%d" % s) if j == 2 else "gmx"
                for half in range(2):
                    for k in range(8):
                        MM(pbank(half)[:], screp[s][:, k, :], wada_t[half][:, k, :], k == 0, k == 7,
                           ["screp%d" % s, "wada%d" % half], ["pb%d" % half])
                    TT("dve", dst[:, half * 512:(half + 1) * 512], pbank(half)[:], bada_b[:, half * 512:(half + 1) * 512], ALU.add,
                       ["pb%d" % half, "bada_b"], [dkey])
                if j == 5:
                    DMA(g2d[s:s + 1, :], gmx[0:1, :], ["gmx"], ["g2d"])
    make_scl(0, 1, O_N1)
    make_scl(1, 4, O_N2)
    for cc in range(INW // 512):
        DMA(w_in_b[:, :, cc * 512:(cc + 1) * 512], w_in[:, cc * 512:(cc + 1) * 512].rearrange("(k p) n -> p k n", p=128),
            [], ["w_in_b"], eng="pool")
    for cc in range(2):
        DMA(w_out_b[:, :, cc * 512:(cc + 1) * 512], w_out[:, cc * 512:(cc + 1) * 512].rearrange("(k p) n -> p k n", p=128),
            [], ["w_out_b"], eng="pool")
    l0 = stgT[:, O_LB:O_LB + 4]
    l1 = stgT[:, O_LB + 4:O_LB + 8]
    TT("dve", lbo[:, 4:8], l1, l0, ALU.subtract, ["stgT"], ["lbo"])
    ACT(lbo[:, 4:8], lbo[:, 4:8], AF.Exp, ["lbo"], ["lbo"])
    TS("dve", lbo[:, 4:8], lbo[:, 4:8], 1.0, None, ALU.add, None, ["lbo"], ["lbo"])
    RECIP(lbo[:, 0:4], lbo[:, 4:8], ["lbo"], ["lbo"])
    TS("dve", lbo[:, 4:8], lbo[:, 0:4], -1.0, 1.0, ALU.mult, ALU.add, ["lbo"], ["lbo"])
    DMA(gnw_b[:], gnw.partition_broadcast(128), [], ["gnw_b"])
    DMA(sub_b[:], subw.partition_broadcast(128), [], ["sub_b"])
    TS("dve", sub_b[:], sub_b[:], 0.8, None, ALU.mult, None, ["sub_b"], ["sub_b"])
    for i, v in enumerate((lq1, lk1, lq2, lk2)):
        DMA(lam4[:, i, :], v.partition_broadcast(128), [], ["lam4"])
    TT("dve", lam4[:, 0, :], lam4[:, 0, :], lam4[:, 1, :], ALU.mult, ["lam4"], ["lam4"])
    TT("dve", lam4[:, 2, :], lam4[:, 2, :], lam4[:, 3, :], ALU.mult, ["lam4"], ["lam4"])
    P.op("dve", lambda e: e.reduce_sum(out=lamv[:, 0:1], in_=lam4[:, 0, :], axis=AX.X), ["lam4"], ["lamv"])
    P.op("dve", lambda e: e.reduce_sum(out=lamv[:, 1:2], in_=lam4[:, 2, :], axis=AX.X), ["lam4"], ["lamv"])
    ACT(lamv[:, 0:2], lamv[:, 0:2], AF.Exp, ["lamv"], ["lamv"])
    TT("dve", neglam[:], lamv[:, 1:2], lamv[:, 0:1], ALU.subtract, ["lamv"], ["neglam"])
    TS("dve", neglam[:], neglam[:], -0.2, None, ALU.add, None, ["neglam"], ["neglam"])
    P.op("pool", lambda e: e.iota(iot[:], pattern=[[128, 16]], base=0, channel_multiplier=1, allow_small_or_imprecise_dtypes=True), [], ["iot"])
    W_H = [128, min(512, TB), min(512, TB), min(512, TB)]
    OFF_H = [15, 12, 12, 12]
    for h in range(4):
        slope = 2.0 ** (-2.0 * (h + 1))
        TS("dve", abias[:, h, :], iot[:], -128.0 * OFF_H[h], slope, ALU.add, ALU.mult, ["iot"], ["abias"])
    MEMSET("pool", vaug[:, :, :, 128:130], 1.0, ["vaug"])
    MEMSET("pool", scanmask[:], 1.0, ["scanmask"])
    MEMSET("pool", scanmask[:].rearrange("p (c t) -> p c t", t=64)[:, :, 0:1], 0.0, ["scanmask"])
    CP("dve", mask2[0:64, :], mask_f[0:64, 0:64], ["mask_f"], ["mask2"])
    CP("dve", mask2[64:128, :], mask_f[64:128, 64:128], ["mask_f"], ["mask2"])
    P.fence()
    MARK('setup_done')

    def post_norm_T(wb, wkey, extra, ekey, dst):
        TT("dve", o_sq, o_f[:], o_f[:], ALU.mult, ["o_f"], ["sgtmp"])
        P.op("dve", lambda e: e.reduce_sum(out=ssq[:, 4:4 + NSUB], in_=o_sq, axis=AX.X), ["sgtmp"], ["ssq%d" % c for c in range(4, 4 + NSUB)])
        norm_rstd(4, NSUB, 128)
        TT("dve", o_sq, o_f[:], rstd[:, 4:4 + NSUB].unsqueeze(2).to_broadcast([128, NSUB, 128]), ALU.mult,
           ["o_f"] + ["rstd%d" % c for c in range(4, 4 + NSUB)], ["sgtmp"])
        if extra is None:
            TT("dve", o_b[:], o_sq, wb[:].unsqueeze(1).to_broadcast([128, NSUB, 128]), ALU.mult, ["sgtmp", wkey], ["o_b"])
        else:
            TT("dve", o_sq, o_sq, wb[:].unsqueeze(1).to_broadcast([128, NSUB, 128]), ALU.mult, ["sgtmp", wkey], ["sgtmp"])
            TT("dve", o_b[:], o_sq, extra, ALU.mult, ["sgtmp", ekey], ["o_b"])
        for q in range(NSUB):
            TR(pbank(4)[:].bitcast(BF16)[:, q * 128:(q + 1) * 128], o_b[:, q, :], ident_b[:], ["o_b", "ident_b"], ["pb4"])
        CP("act", dst, pbank(4)[:].bitcast(BF16)[:, 0:NSUB * 128], ["pb4"], ["mixT"])

    HQ, HF, HI, HG, AQ, AK, AV = 0, 512, 1024, 1536, 2048, 2560, 3072
    pb_rr = [0]

    def next_pb():
        pb_rr[0] ^= 1
        return pb_rr[0]

    def proj_feat(uT_, ukey, col0, consume):
        b = next_pb()
        for k in range(8):
            MM(pbank(b)[:, 0:TB], w_in_b[:, k, col0:col0 + 128], uT_[:, k, :], k == 0, k == 7, ["w_in_b", ukey], ["pb%d" % b])
        consume(pbank(b)[:, 0:TB], "pb%d" % b)

    def proj_tok(uT_, ukey, col0, sub, consume):
        b = next_pb()
        for k in range(8):
            MM(pbank(b)[:], uT_[:, k, sub * 128:(sub + 1) * 128], w_in_b[:, k, col0:col0 + 512], k == 0, k == 7, ["w_in_b", ukey], ["pb%d" % b])
        consume(pbank(b)[:], "pb%d" % b)

    blocks = [(s, blk) for s in range(NSEQ) for blk in range(NBLK)]

    def emit_norm(bi):
        s, blk = blocks[bi]
        tok0 = s * T + blk * TB
        uT_ = uT2[bi % 2]
        ukey = "uT%d" % (bi % 2)
        for sub in range(NSUB):
            xb = xt[sub % 2]
            DMA(xb[:], x[tok0 + sub * 128: tok0 + (sub + 1) * 128, :], [], ["xt%d" % (sub % 2)])
            norm_stats(xb[:], "xt%d" % (sub % 2), sub % 2)
            norm_rstd(sub % 2, 1, D)
            norm_apply_T(xb[:], "xt%d" % (sub % 2), sub % 2, s, 0, 0, uT_, ukey, sub * 128)

    emit_norm(0)
    for bi, (s, blk) in enumerate(blocks):
        tok0 = s * T + blk * TB
        uT = uT2[bi % 2]
        ukey = "uT%d" % (bi % 2)
        if blk == 0:
            MEMSET("pool", S[:], 0.0, ["S"])
        for h in range(4):
            proj_feat(uT, ukey, AQ + h * 128, lambda ps, key, h=h: CP("act", qTa[h][:], ps, [key], ["qTa%d" % h]))
            proj_feat(uT, ukey, AK + h * 128, lambda ps, key, h=h: CP("dve", kT[h][:, blk * TB:(blk + 1) * TB], ps, [key], ["kT%d" % h]))
        for sub in range(NSUB):
            kb = blk * NSUB + sub
            proj_tok(uT, ukey, AV, sub, lambda ps, key, kb=kb: CP("act", vaug[:, kb, :, 0:128], ps.rearrange("p (h v) -> p h v", v=128), [key], ["vaug"]))
        MARK('attnproj_done')
        for sub in range(NSUB):
            proj_tok(uT, ukey, HI, sub, lambda ps, key, sub=sub: CP("act", v_tok[:, sub, :], ps, [key], ["v_tok"]))

            def gate(ps, key, sub=sub):
                ACT(sgtmp[:], ps, AF.Exp, [key], ["sgtmp"], scale=-1.0)
                ACT(sgtmp[:], sgtmp[:], AF.Ln, ["sgtmp"], ["sgtmp"], bias=1.0)
                ACT(sgtmp[:], sgtmp[:], AF.Exp, ["sgtmp"], ["sgtmp"], scale=-1.0)
                TT("dve", sg_tok[:, sub, :], ps, sgtmp[:], ALU.mult, [key, "sgtmp"], ["sg_tok"])
            proj_tok(uT, ukey, HG, sub, gate)
        MARK('vgate_done')
        for h in range(4):
            W = W_H[h]
            nqt = TB // W
            last_kb = blk * NSUB + NSUB - 1
            tiles = []
            for kb in range(last_kb + 1):
                for m in range(2):
                    for qt in range(nqt):
                        gq0 = blk * TB + qt * W
                        if kb * 128 >= gq0 + W:
                            continue
                        lo = max(gq0, kb * 128)
                        tiles.append((kb, m, lo - blk * TB, gq0 + W - lo, kb - gq0 // 128 + OFF_H[h], lo == kb * 128))
            acc_started = set()
            LA = int(_os2.environ.get('KLA', '2'))
            SB = [2, 3, 0, 1]
            for i in range(len(tiles) + LA):
                if i < len(tiles):
                    kb, m, lq, nq, d, diag = tiles[i]
                    sb_ = SB[i % 4]
                    pt = PT[i % NPT]
                    ptk = "PT%d" % (i % NPT)
                    MM(pbank(sb_)[:, 0:nq], kT[h][m * 64:(m + 1) * 64, kb * 128:(kb + 1) * 128],
                       qTa[h][m * 64:(m + 1) * 64, lq:lq + nq], True, True, ["kT%d" % h, "qTa%d" % h], ["pb%d" % sb_])
                    ACT(pt[:, 0:nq], pbank(sb_)[:, 0:nq], AF.Exp, ["pb%d" % sb_, "abias"], [ptk], scale=0.125, bias=abias[:, h, d:d + 1])
                    if diag:
                        TT("pool", pt[:, 0:128], pt[:, 0:128], mask_b[:], ALU.mult, [ptk, "mask_b"], [ptk])
                ip = i - LA
                if ip >= 0:
                    kb, m, lq, nq, d, diag = tiles[ip]
                    pt = PT[ip % NPT]
                    ptk = "PT%d" % (ip % NPT)
                    for j in range(nq // 128):
                        qb = (lq // 128) + j
                        a = m * NSUB + qb
                        ab, asl = 5 + a // 3, (a % 3) * 130
                        first = ab not in acc_started
                        acc_started.add(ab)
                        MM(pbank(ab)[:, asl:asl + 130], pt[:, j * 128:(j + 1) * 128], vaug[:, kb, h, :], first,
                           kb == blk * NSUB + qb, [ptk, "vaug"], ["pb%d" % ab], skip_group_check=True)
            for a in range(2 * NSUB):
                accp = pbank(5 + a // 3)[:, (a % 3) * 130:(a % 3) * 130 + 130]
                CP("act", acc_sb[:, a, :], accp, ["pb%d" % (5 + a // 3)], ["acc_sb"])
            RECIP(rl[:], acc_sb[:, :, 128], ["acc_sb"], ["rl"])
            TS("dve", rl[:, NSUB:2 * NSUB], rl[:, NSUB:2 * NSUB], neglam[:, 0:1], None, ALU.mult, None, ["rl", "neglam"], ["rl"])
            TT("dve", acc_sb[:, :, 0:128], acc_sb[:, :, 0:128], rl[:].unsqueeze(2).to_broadcast([128, 2 * NSUB, 128]), ALU.mult,
               ["acc_sb", "rl"], ["acc_sb"])
            TT("dve", o_f[:], acc_sb[:, 0:NSUB, 0:128], acc_sb[:, NSUB:2 * NSUB, 0:128], ALU.add, ["acc_sb"], ["o_f"])
            post_norm_T(sub_b, "sub_b", None, None, mixT[:, 4 + h, :])
        MARK('attn_done')
        if bi + 1 < len(blocks):
            emit_norm(bi + 1)
        def prep(h):
            i = h % 2

            def fchain(ps, key):
                ACT(t_sig[:], ps, AF.Exp, [key], ["t_sig"], scale=-1.0)
                ACT(t_sig[:], t_sig[:], AF.Ln, ["t_sig"], ["t_sig"], bias=1.0)
                ACT(t_sig[:], t_sig[:], AF.Exp, ["t_sig"], ["t_sig"], scale=-1.0)
                TS("dve", t_sig[:], t_sig[:], lbo[:, 4 + h:5 + h], lbo[:, h:h + 1], ALU.mult, ALU.add, ["t_sig", "lbo"], ["t_sig"])
                ACT(t_lf[:], t_sig[:], AF.Ln, ["t_sig"], ["t_lf"])
                TS("pool", t_omf[:], t_sig[:], -1.0, 1.0, ALU.mult, ALU.add, ["t_sig"], ["t_omf"])
                P.op("dve", lambda e: e.tensor_tensor_scan(out=t_b[:], data0=scanmask[:], data1=t_lf[:], initial=0.0,
                                                           op0=ALU.mult, op1=ALU.add), ["scanmask", "t_lf"], ["t_b"])
                b3 = t_b[:].rearrange("p (c t) -> p c t", t=64)
                ACT(erbl[i][:], b3[:, :, 31:64:32], AF.Exp, ["t_b"], ["erbl%d" % i])
                TT("dve", t_lf[:].rearrange("p (c t) -> p c t", t=64), b3, b3[:, :, 31:32].to_broadcast([128, NCH, 64]), ALU.subtract,
                   ["t_b"], ["t_lf"])
                ACT(t_E[:], t_lf[:], AF.Exp, ["t_lf"], ["t_E"])
                CP("pool", elast[i][:], t_E[:].rearrange("p (c t) -> p c t", t=64)[:, :, 63], ["t_E"], ["elast%d" % i])
                ACT(t_Ei[:], t_lf[:], AF.Exp, ["t_lf"], ["t_Ei"], scale=-1.0)
                TT("pool", ktilT[i][:], t_omf[:], t_Ei[:], ALU.mult, ["t_omf", "t_Ei"], ["ktilT%d" % i])
            proj_feat(uT, ukey, HF + h * 128, fchain)
            proj_feat(uT, ukey, HQ + h * 128, lambda ps, key: TT("dve", qtil[i][:], ps, t_E[:], ALU.mult, [key, "t_E"], ["qtil%d" % i]))
            for sub in range(NSUB):
                TR(pbank(4)[:].bitcast(BF16)[:, 0:128], ktilT[i][:, sub * 128:(sub + 1) * 128], ident_b[:], ["ktilT%d" % i, "ident_b"], ["pb4"])
                CP("act", ktil_tok[i][:, sub, :], pbank(4)[:].bitcast(BF16)[:, 0:128], ["pb4"], ["ktil_tok%d" % i])

        def rec(h):
            i = h % 2
            qk, kk, tk = "qtil%d" % i, "ktilT%d" % i, "ktil_tok%d" % i
            for sub in range(NSUB):
                ob = 3 if sub % 2 == 0 else 7
                okey = "pb%d" % ob
                kvt, AT_sb = kvt2[sub % 2], AT_sb2[sub % 2]
                kvk, atk = "kvt%d" % (sub % 2), "AT_sb%d" % (sub % 2)
                for half in range(2):
                    c = sub * 2 + half
                    r0 = half * 64
                    cs = slice(c * 64, (c + 1) * 64)
                    atb = 5 if half == 0 else 2
                    MM(pbank(atb)[r0:r0 + 64, 0:64], ktilT[i][:, cs], qtil[i][:, cs], True, True, [kk, qk], ["pb%d" % atb], tile_position=(0, r0))
                for half in range(2):
                    r0 = half * 64
                    kvb = 6 if half == 0 else 2
                    MM(pbank(kvb)[:, 128:256], ktil_tok[i][r0:r0 + 64, sub, :], v_tok[r0:r0 + 64, sub, h * 128:(h + 1) * 128],
                       True, True, [tk, "v_tok"], ["pb%d" % kvb])
                for half in range(2):
                    atb = 5 if half == 0 else 2
                    r0 = half * 64
                    TT("dve", AT_sb[r0:r0 + 64, :], pbank(atb)[r0:r0 + 64, 0:64], mask2[r0:r0 + 64, :], ALU.mult, ["pb%d" % atb, "mask2"], [atk])
                for half in range(2):
                    kvb = 6 if half == 0 else 2
                    TS("dve", kvt[:, half, :], pbank(kvb)[:, 128:256], elast[i][:, 2 * sub + half:2 * sub + half + 1], None, ALU.mult, None,
                       ["pb%d" % kvb, "elast%d" % i], [kvk])
                for half in range(2):
                    c = sub * 2 + half
                    r0 = half * 64
                    cs = slice(c * 64, (c + 1) * 64)
                    TS("dve", Sr[:, half, :], S[:, h, :], erbl[i][:, c, 0:1], None, ALU.mult, None, ["S", "erbl%d" % i], ["Sr%d" % half])
                    MM(pbank(ob)[r0:r0 + 64, 0:128], qtil[i][:, cs], Sr[:, half, :], True, False, [qk, "Sr%d" % half], [okey], tile_position=(0, r0))
                    MM(pbank(ob)[r0:r0 + 64, 0:128], AT_sb[r0:r0 + 64, :], v_tok[r0:r0 + 64, sub, h * 128:(h + 1) * 128], False, True,
                       [atk, "v_tok"], [okey], tile_position=(r0, r0))
                    STT("dve", S[:, h, :], S[:, h, :], erbl[i][:, c, 1:2], kvt[:, half, :], ALU.mult, ALU.add, ["S", "erbl%d" % i, kvk], ["S"])
                CP("act", o_f[:, sub, :], pbank(ob)[:, 0:128], [okey], ["o_f"])
            post_norm_T(gnw_b, "gnw_b", sg_tok[:, :, h * 128:(h + 1) * 128], "sg_tok", mixT[:, h, :])

        MARK('norm_next_done')
        prep(0)
        prep(1)
        MARK('prep01_done')
        rec(0)
        MARK('rec0_done')
        prep(2)
        rec(1)
        prep(3)
        rec(2)
        rec(3)
        MARK('hgrn_done')
        for sub in range(NSUB):
            DMA(hres[:], x[tok0 + sub * 128: tok0 + (sub + 1) * 128, :], [], ["hres"])
            for half in range(2):
                b = 5 + half
                for k in range(8):
                    MM(pbank(b)[:], mixT[:, k, sub * 128:(sub + 1) * 128], w_out_b[:, k, half * 512:(half + 1) * 512], k == 0, k == 7,
                       ["mixT", "w_out_b"], ["pb%d" % b])
                TT("dve", gmx[:, half * 512:(half + 1) * 512], pbank(b)[:], gb[s][:, half * 512:(half + 1) * 512], ALU.mult,
                   ["pb%d" % b, "gb%d" % s], ["gmx"])
            TT("pool", hres[:], hres[:], gmx[:], ALU.add, ["hres", "gmx"], ["hres"])
            DMA(h1[tok0 + sub * 128: tok0 + (sub + 1) * 128, :], hres[:], ["hres"], ["h1dram"])
    P.fence()

    MARK('phaseA_done')
    NSB = TBB // 128
    A.off = phase_base
    w_g_b = A.alloc("w_g_b", [128, 8, DFF], BF16)
    w_u_b = A.alloc("w_u_b", [128, 8, DFF], BF16)
    w_d_b = A.alloc("w_d_b", [128, NKF, D], BF16)
    fnw_b = A.alloc("fnw_b", [128, D], F32)
    g2b = A.alloc("g2b", [128, D], F32)
    ht = [A.alloc("ht%d" % i, [128, D], F32) for i in range(NSB)]
    u2T = A.alloc("u2T", [128, 8, TBB], BF16)
    hidT = A.alloc("hidT", [128, NKF, TBB], BF16)
    sgb_all = A.alloc("sgb_all", [128, 2, 512], F32)
    sgb = [sgb_all[:, i, 0:TBB] for i in range(2)]
    sq_junk = sgb_all[:].rearrange("p a b -> p (a b)")
    h2 = [A.alloc("h2_%d" % i, [128, D], F32) for i in range(2)]
    ffg = A.alloc("ffg", [128, D], F32)

    for cc in range(0, DFF, 512):
        w = min(512, DFF - cc)
        DMA(w_g_b[:, :, cc:cc + w], w_g[:, cc:cc + w].rearrange("(k p) n -> p k n", p=128), [], ["w_g_b%d" % (cc // 512)], eng="pool")
        DMA(w_u_b[:, :, cc:cc + w], w_u[:, cc:cc + w].rearrange("(k p) n -> p k n", p=128), [], ["w_u_b%d" % (cc // 512)], eng="pool")
    for k2 in range(0, NKF, 2):
        DMA(w_d_b[:, k2:k2 + 2, :], w_d[k2 * 128:(k2 + 2) * 128, :].rearrange("(k p) n -> p k n", p=128), [], ["w_d_b%d" % (k2 // 2)], eng="pool")
    DMA(fnw_b[:], fnw.partition_broadcast(128), [], ["fnw_b"])

    bblocks = [(s, blk) for s in range(NSEQ) for blk in range(T // TBB)]

    def b_norm_stats(bi):
        s, blk = bblocks[bi]
        tok0 = s * T + blk * TBB
        for sub in range(NSB):
            DMA(ht[sub][:], h1[tok0 + sub * 128: tok0 + (sub + 1) * 128, :], ["h1dram"], ["ht%d" % sub])
            ACT(sq_junk, ht[sub][:], AF.Square, ["ht%d" % sub], ["sgb0", "sgb1", "ssq%d" % sub], accum_out=ssq[:, sub:sub + 1])
        norm_rstd(0, NSB, D)
        for sub in range(NSB):
            TS("dve", ht[sub][:], ht[sub][:], rstd[:, sub:sub + 1], None, ALU.mult, None, ["ht%d" % sub, "rstd%d" % sub], ["ht%d" % sub])

    def b_norm_T(bi):
        s, blk = bblocks[bi]
        for sub in range(NSB):
            src_tile, key = ht[sub][:], "ht%d" % sub
            for g in range(2):
                for k in range(g * 4, g * 4 + 4):
                    TR(pbank(4)[:, (k % 4) * 128:(k % 4 + 1) * 128], src_tile[:, k * 128:(k + 1) * 128], ident_f[:], [key, "ident_f"], ["pb4"])
                for k in range(g * 4, g * 4 + 4):
                    ACT(u2T[:, k, sub * 128:(sub + 1) * 128], pbank(4)[:, (k % 4) * 128:(k % 4 + 1) * 128], AF.Identity, ["pb4", "scl", "modT"], ["u2T"],
                        scale=scl[:, 1, k, s:s + 1], bias=modT[:, 3, k, s:s + 1])

    def b_norm(bi):
        b_norm_stats(bi)
        b_norm_T(bi)

    def b_down(bi, sub):
        s, blk = bblocks[bi]
        tok0 = s * T + blk * TBB
        hb = h2[sub % 2]
        hk = "h2_%d" % (sub % 2)
        DMA(hb[:], h1[tok0 + sub * 128: tok0 + (sub + 1) * 128, :], ["h1dram"], [hk])
        for half in range(2):
            b = 4 + (2 * sub + half) % 4
            for f in range(NKF):
                MM(pbank(b)[:], hidT[:, f, sub * 128:(sub + 1) * 128], w_d_b[:, f, half * 512:(half + 1) * 512], f == 0, f == NKF - 1,
                   ["hidT", "w_d_b%d" % (f // 2)], ["pb%d" % b])
        for half in range(2):
            b = 4 + (2 * sub + half) % 4
            TT("dve", ffg[:, half * 512:(half + 1) * 512], pbank(b)[:], g2b[:, half * 512:(half + 1) * 512], ALU.mult,
               ["pb%d" % b, "g2b"], ["ffg"])
        TT("pool", hb[:], hb[:], ffg[:], ALU.add, [hk, "ffg"], [hk])
        ACT(ffg[:], hb[:], AF.Square, [hk], ["ffg", "ssq6"], accum_out=ssq[:, 6:7])
        norm_rstd(6, 1, D)
        STT("dve", hb[:], hb[:], rstd[:, 6:7], fnw_b[:], ALU.mult, ALU.mult, [hk, "rstd6", "fnw_b"], [hk])
        DMA(out[tok0 + sub * 128: tok0 + (sub + 1) * 128, :], hb[:], [hk], ["outdram"])

    MARK('phaseB_loads')
    b_norm(0)
    MARK('bnorm0')
    for bi, (s, blk) in enumerate(bblocks):
        if blk == 0:
            DMA(g2b[:], g2d[s, :].partition_broadcast(128), ["g2d"], ["g2b"])
        for f in range(NKF):
            bg, bu = (0, 1) if f % 2 == 0 else (2, 3)
            for k in range(8):
                MM(pbank(bg)[:, 0:TBB], w_g_b[:, k, f * 128:(f + 1) * 128], u2T[:, k, :], k == 0, k == 7, ["w_g_b%d" % (f // 4), "u2T"], ["pb%d" % bg])
            for k in range(8):
                MM(pbank(bu)[:, 0:TBB], w_u_b[:, k, f * 128:(f + 1) * 128], u2T[:, k, :], k == 0, k == 7, ["w_u_b%d" % (f // 4), "u2T"], ["pb%d" % bu])
            ACT(sgb[f % 2], pbank(bg)[:, 0:TBB], AF.Silu, ["pb%d" % bg], ["sgb%d" % (f % 2)])
            TT("dve", hidT[:, f, :], pbank(bu)[:, 0:TBB], sgb[f % 2], ALU.mult, ["pb%d" % bu, "sgb%d" % (f % 2)], ["hidT"])
        MARK('gu_done')
        if bi + 1 < len(bblocks):
            b_norm_stats(bi + 1)
        for sub in range(NSB):
            b_down(bi, sub)
            MARK('down%d' % sub)
            if sub == NSB - 2 and bi + 1 < len(bblocks):
                b_norm_T(bi + 1)

    import os as _os
    _n = int(_os.environ.get('KSTOP', '0'))
    if _n:
        P.ops = P.ops[:_n]
    print('NOPS', len(P.ops))
    P.analyze()
    esems = {e: nc.alloc_semaphore("es_" + e) for e in ENGS}
    dsems = [nc.alloc_semaphore("ds%d" % i) for i in range(P.n_dma_sems)]
    with nc.Block() as block:
        P.emit(block, esems, dsems)
    return nc


def core_inputs(inputs, core, NSEQ, T):
    f = lambda a: np.ascontiguousarray(np.asarray(a, dtype=np.float32))
    sl = slice(core * NSEQ, (core + 1) * NSEQ)
    return {
        "x": f(np.asarray(inputs["x"])[sl].reshape(NSEQ * T, D)),
        "c": f(np.asarray(inputs["c"])[sl].reshape(NSEQ * 8, 128)),
        "w_ada": f(np.asarray(inputs["w_ada"])[0]),
        "b_ada": f(np.asarray(inputs["b_ada"])[0]),
        "norm1_w": f(np.asarray(inputs["norm1_w"])[0].reshape(8, 128)),
        "w_in": f(np.asarray(inputs["w_in"])[0]),
        "lb_logits": f(np.asarray(inputs["hgrn_lb_logits"]).reshape(8, 128)),
        "gnorm_w": f(np.asarray(inputs["hgrn_gnorm_w"])[0]),
        "lq1": f(np.asarray(inputs["diff_lambda_q1"])[0]),
        "lk1": f(np.asarray(inputs["diff_lambda_k1"])[0]),
        "lq2": f(np.asarray(inputs["diff_lambda_q2"])[0]),
        "lk2": f(np.asarray(inputs["diff_lambda_k2"])[0]),
        "subln_w": f(np.asarray(inputs["diff_subln_w"])[0]),
        "w_out": f(np.asarray(inputs["w_out"])[0]),
        "norm2_w": f(np.asarray(inputs["norm2_w"])[0].reshape(8, 128)),
        "w_g": f(np.asarray(inputs["w_ffn_gate"])[0]),
        "w_u": f(np.asarray(inputs["w_ffn_up"])[0]),
        "w_d": f(np.asarray(inputs["w_ffn_down"])[0]),
        "fnw": f(np.asarray(inputs["final_norm_w"])),
    }


def kernel(**inputs):
    B, T, _ = np.asarray(inputs["x"]).shape
    n = 8
    NSEQ = B // n
    nc = build(T=T, NSEQ=NSEQ)
    in_maps = [core_inputs(inputs, i, NSEQ, T) for i in range(n)]
    res = run_bass_kernel_spmd(nc, in_maps, core_ids=list(range(n)))
    outs = [np.asarray(r["out"]).reshape(NSEQ, T, D) for r in res.results]
    return np.concatenate(outs, axis=0).astype(np.float32)
```
